# Optimizing a Trainium2 kernel written in Bass

```python
import math
import jax, jax.numpy as jnp
from jax import lax
import numpy as np

D_MODEL = 1024
BATCH = 4
SEQ = 8192
DEPTH = 1

CHUNK = 64
Q_BLOCK = 128
N_MEM = 256
HEAD_DIM = 64
SB_HEADS = 8
SB_WIDTH = SB_HEADS * HEAD_DIM
HG_HEADS = 4
HG_KEY_DIM = 128
HG_VAL_DIM = 64
HG_KWIDTH = HG_HEADS * HG_KEY_DIM
HG_VWIDTH = HG_HEADS * HG_VAL_DIM
MEM_HEADS = 4
MEM_WIDTH = MEM_HEADS * HEAD_DIM
MIX_WIDTH = SB_WIDTH + HG_VWIDTH + MEM_WIDTH
SPLIT_SIZES = (SB_WIDTH, SB_WIDTH, SB_WIDTH, HG_KWIDTH, HG_KWIDTH, HG_VWIDTH, HG_VWIDTH, MEM_WIDTH)
IN_WIDTH = 3 * SB_WIDTH + 2 * HG_KWIDTH + 2 * HG_VWIDTH + MEM_WIDTH
PEER_HEADS = 8
N_KEYS = 128
N_EXPERTS = N_KEYS * N_KEYS
PEER_TOPK = 16
D_KEY = 256
HALF_KEY = D_KEY // 2
PEER_TOKEN_BLOCK = 128
EPS = 1e-6

kernel_name = "hybrid_stickbreak_hgrn2_memattn_peer"


def rms_norm(x, gain):
    xf = x.astype(jnp.float32)
    y = xf * lax.rsqrt(jnp.mean(xf * xf, axis=-1, keepdims=True) + EPS)
    return (y * gain.astype(jnp.float32)).astype(x.dtype)


def stick_breaking_attention(q, k, v):
    b, s, h, d = q.shape
    nb = s // Q_BLOCK
    qb = q.reshape(b, nb, Q_BLOCK, h, d).transpose(1, 0, 3, 2, 4)
    kh = k.transpose(0, 2, 1, 3)
    vh = v.transpose(0, 2, 1, 3)
    key_pos = jnp.arange(s)
    scale = 1.0 / math.sqrt(d)

    def block(args):
        q_blk, i = args
        q_pos = i * Q_BLOCK + jnp.arange(Q_BLOCK)
        z = jnp.einsum('bhqd,bhkd->bhqk', q_blk, kh).astype(jnp.float32) * scale
        mask = key_pos[None, :] < q_pos[:, None]
        log_1m = jnp.where(mask, jax.nn.log_sigmoid(-z), 0.0)
        tail = lax.cumsum(log_1m, axis=3, reverse=True) - log_1m
        log_a = jax.nn.log_sigmoid(z) + tail
        a = jnp.where(mask, jnp.exp(log_a), 0.0)
        return jnp.einsum('bhqk,bhkd->bhqd', a.astype(vh.dtype), vh)

    out = lax.map(block, (qb, jnp.arange(nb)))
    return out.transpose(1, 0, 3, 2, 4).reshape(b, s, h, d)


def hgrn2_recurrence(q, f_logit, i, lower_bound):
    b, s, h, dk = q.shape
    dv = i.shape[-1]
    nc = s // CHUNK
    f = lower_bound + (1.0 - lower_bound) * jax.nn.sigmoid(f_logit.astype(jnp.float32))
    log_f = jnp.log(f)
    k = 1.0 - f
    qf = jax.nn.silu(q.astype(jnp.float32))
    vf = i.astype(jnp.float32)

    def to_chunks(t):
        return t.reshape(b, nc, CHUNK, h, t.shape[-1]).transpose(1, 0, 3, 2, 4)

    causal = jnp.tril(jnp.ones((CHUNK, CHUNK), dtype=bool))[:, :, None]

    def step(state, inp):
        qc, kc, vc, gc = inp
        bcum = jnp.cumsum(gc, axis=2)
        diff = bcum[:, :, :, None, :] - bcum[:, :, None, :, :]
        decay = jnp.where(causal, jnp.exp(jnp.where(causal, diff, 0.0)), 0.0)
        scores = jnp.einsum('bhtk,bhtsk,bhsk->bhts', qc, decay, kc)
        intra = jnp.einsum('bhts,bhsv->bhtv', scores, vc)
        inter = jnp.einsum('bhtk,bhkv->bhtv', qc * jnp.exp(bcum), state)
        last = bcum[:, :, -1:, :]
        new_state = (jnp.exp(last[:, :, 0, :])[..., None] * state
                     + jnp.einsum('bhsk,bhsv->bhkv', kc * jnp.exp(last - bcum), vc))
        return new_state, intra + inter

    state0 = jnp.zeros((b, h, dk, dv), jnp.float32)
    _, out = lax.scan(step, state0, (to_chunks(qf), to_chunks(k), to_chunks(vf), to_chunks(log_f)))
    return out.transpose(1, 0, 3, 2, 4).reshape(b, s, h, dv)


def memory_attention(q, mem_k, mem_v, q_gain, k_gain):
    qn = rms_norm(q, q_gain)
    kn = rms_norm(mem_k, k_gain)
    scale = 1.0 / math.sqrt(q.shape[-1])
    logits = jnp.einsum('bshd,bmhd->bhsm', qn, kn).astype(jnp.float32) * scale
    p = jax.nn.softmax(logits, axis=-1)
    return jnp.einsum('bhsm,bmhd->bshd', p.astype(mem_v.dtype), mem_v)


def peer_ffn(h, w_query, sub_keys, expert_u, expert_v):
    b, s, d = h.shape
    nb = s // PEER_TOKEN_BLOCK
    hb = h.reshape(b, nb, PEER_TOKEN_BLOCK, d).transpose(1, 0, 2, 3)

    def block(x_blk):
        t = x_blk.shape[1]
        q = (x_blk @ w_query).reshape(b, t, PEER_HEADS, 2, HALF_KEY)
        scores = jnp.einsum('bthcd,hcnd->bthcn', q, sub_keys).astype(jnp.float32)
        top_s, top_i = lax.top_k(scores, PEER_TOPK)
        cand_s = top_s[..., 0, :, None] + top_s[..., 1, None, :]
        cand_i = top_i[..., 0, :, None] * N_KEYS + top_i[..., 1, None, :]
        cand_s = cand_s.reshape(b, t, PEER_HEADS, PEER_TOPK * PEER_TOPK)
        cand_i = cand_i.reshape(b, t, PEER_HEADS, PEER_TOPK * PEER_TOPK)
        best_s, pos = lax.top_k(cand_s, PEER_TOPK)
        expert_idx = jnp.take_along_axis(cand_i, pos, axis=-1)
        gate = jax.nn.softmax(best_s, axis=-1)
        u = expert_u[expert_idx]
        v = expert_v[expert_idx]
        act = jax.nn.gelu(jnp.einsum('btd,bthkd->bthk', x_blk, u).astype(jnp.float32), approximate=False)
        return jnp.einsum('bthk,bthkd->btd', (gate * act).astype(v.dtype), v)

    out = lax.map(block, hb)
    return out.transpose(1, 0, 2, 3).reshape(b, s, d)


def setup_inputs(seed: int = 0) -> dict:
    key = jax.random.key(seed)
    ks = jax.random.split(key, 20)
    f32 = jnp.float32
    nrm = lambda k, shape, scale: jax.random.normal(k, shape, f32) * scale
    gain = lambda k, shape: 1.0 + 0.02 * jax.random.normal(k, shape, f32)
    return {
        "x": jax.random.normal(ks[0], (BATCH, SEQ, D_MODEL), f32),
        "mem": jax.random.normal(ks[1], (BATCH, N_MEM, D_MODEL), f32),
        "norm_mix_gain": gain(ks[2], (DEPTH, D_MODEL)),
        "w_in": nrm(ks[3], (DEPTH, D_MODEL, IN_WIDTH), D_MODEL ** -0.5),
        "gamma_lb": nrm(ks[4], (DEPTH + 1, HG_KWIDTH), 0.5),
        "sb_out_gain": gain(ks[5], (DEPTH, SB_HEADS, HEAD_DIM)),
        "hg_out_gain": gain(ks[6], (DEPTH, HG_HEADS, HG_VAL_DIM)),
        "mem_norm_gain": gain(ks[7], (D_MODEL,)),
        "w_mem_kv": nrm(ks[8], (DEPTH, D_MODEL, 2 * MEM_WIDTH), D_MODEL ** -0.5),
        "mem_q_gain": gain(ks[9], (DEPTH, HEAD_DIM)),
        "mem_k_gain": gain(ks[10], (DEPTH, HEAD_DIM)),
        "w_out": nrm(ks[11], (DEPTH, MIX_WIDTH, D_MODEL), MIX_WIDTH ** -0.5),
        "norm_ffn_gain": gain(ks[12], (DEPTH, D_MODEL)),
        "peer_w_query": nrm(ks[13], (DEPTH, D_MODEL, PEER_HEADS * D_KEY), D_MODEL ** -0.5),
        "peer_sub_keys": nrm(ks[14], (DEPTH, PEER_HEADS, 2, N_KEYS, HALF_KEY), HALF_KEY ** -0.5),
        "peer_u": nrm(ks[15], (DEPTH, N_EXPERTS, D_MODEL), D_MODEL ** -0.5),
        "peer_v": nrm(ks[16], (DEPTH, N_EXPERTS, D_MODEL), PEER_HEADS ** -0.5),
    }


def reference(x, mem, norm_mix_gain, w_in, gamma_lb, sb_out_gain, hg_out_gain, mem_norm_gain,
              w_mem_kv, mem_q_gain, mem_k_gain, w_out, norm_ffn_gain, peer_w_query,
              peer_sub_keys, peer_u, peer_v):
    b, s, _ = x.shape
    m = mem.shape[1]
    lb_all = jnp.cumsum(jax.nn.softmax(gamma_lb.astype(jnp.float32), axis=0), axis=0)
    mem_n = rms_norm(mem, mem_norm_gain)
    split_points = [int(v) for v in np.cumsum(SPLIT_SIZES)[:-1]]
    for l in range(DEPTH):
        h = rms_norm(x, norm_mix_gain[l])
        proj = h @ w_in[l]
        sb_q, sb_k, sb_v, hg_q, hg_f, hg_i, hg_g, mem_q = jnp.split(proj, split_points, axis=-1)

        sb_o = stick_breaking_attention(sb_q.reshape(b, s, SB_HEADS, HEAD_DIM),
                                        sb_k.reshape(b, s, SB_HEADS, HEAD_DIM),
                                        sb_v.reshape(b, s, SB_HEADS, HEAD_DIM))
        sb_o = rms_norm(sb_o, sb_out_gain[l]).reshape(b, s, SB_WIDTH)

        lb = lb_all[l].reshape(HG_HEADS, HG_KEY_DIM)
        hg_o = hgrn2_recurrence(hg_q.reshape(b, s, HG_HEADS, HG_KEY_DIM),
                                hg_f.reshape(b, s, HG_HEADS, HG_KEY_DIM),
                                hg_i.reshape(b, s, HG_HEADS, HG_VAL_DIM), lb).astype(x.dtype)
        hg_o = rms_norm(hg_o, hg_out_gain[l]).reshape(b, s, HG_VWIDTH) * jax.nn.silu(hg_g)

        mem_kv = mem_n @ w_mem_kv[l]
        mem_k, mem_v = jnp.split(mem_kv, 2, axis=-1)
        mem_o = memory_attention(mem_q.reshape(b, s, MEM_HEADS, HEAD_DIM),
                                 mem_k.reshape(b, m, MEM_HEADS, HEAD_DIM),
                                 mem_v.reshape(b, m, MEM_HEADS, HEAD_DIM),
                                 mem_q_gain[l], mem_k_gain[l]).reshape(b, s, MEM_WIDTH)

        mixed = jnp.concatenate([sb_o, hg_o, mem_o], axis=-1)
        x = x + mixed @ w_out[l]

        x = x + peer_ffn(rms_norm(x, norm_ffn_gain[l]), peer_w_query[l], peer_sub_keys[l],
                         peer_u[l], peer_v[l])
    return x
```

```python
import numpy as np
from contextlib import ExitStack
import concourse.bass as bass
import concourse.mybir as mybir
from concourse.bass_utils import run_bass_kernel_spmd

F32 = mybir.dt.float32
BF16 = mybir.dt.bfloat16
U32 = mybir.dt.uint32
I32 = mybir.dt.int32
ALU = mybir.AluOpType
AF = mybir.ActivationFunctionType
AX = mybir.AxisListType

D = 1024
NCH = 8
EPS = 1e-6
N_EXP = 16384
NEG = -1.0e30


class Reg:
    __slots__ = ("name", "w", "r", "sem", "cnt")

    def __init__(self, name):
        self.name = name
        self.w = None
        self.r = {}
        self.sem = None
        self.cnt = 0


class Tile:
    def __init__(self, t, name, view=None):
        self.t = t
        self.v = view
        self.r = Reg(name)

    def __getitem__(self, k):
        if self.v is not None:
            return self.v[k]
        return self.t[k]


class RR:
    def __init__(self, tiles):
        self.tiles = tiles
        self.i = 0

    def next(self):
        t = self.tiles[self.i % len(self.tiles)]
        self.i += 1
        return t


class TK:
    def __init__(self, nc):
        self.nc = nc
        self.eng = {"pe": nc.tensor, "act": nc.scalar, "dve": nc.vector,
                    "pool": nc.gpsimd, "sp": nc.sync}
        self.sems = []
        self.issued = []
        self.own = {}
        for k in ("pe", "act", "dve", "pool"):
            self.own[k] = self._newsem("s_" + k)
        self.waited = {k: {} for k in self.eng}

    def _newsem(self, name):
        self.sems.append(self.nc.alloc_semaphore(name))
        self.issued.append(0)
        return len(self.sems) - 1

    def _deps(self, reads, writes):
        deps = {}

        def add(k, v):
            if deps.get(k, 0) < v:
                deps[k] = v
        for t in reads:
            if t.w is not None:
                add(*t.w)
        for t in writes:
            if t.w is not None:
                add(*t.w)
            for k, v in t.r.items():
                add(k, v)
        return deps

    def _dowaits(self, e, deps):
        own = self.own.get(e)
        for k, v in deps.items():
            if k == own and e == "pe":
                continue
            if self.waited[e].get(k, 0) >= v:
                continue
            self.eng[e].wait_ge(self.sems[k], v)
            self.waited[e][k] = v

    def _mark(self, tok, reads, writes):
        k, v = tok
        for t in reads:
            if t.r.get(k, 0) < v:
                t.r[k] = v
        for t in writes:
            t.w = tok
            t.r = {}

    def op(self, e, fn, reads=(), writes=()):
        reads = [x.r if isinstance(x, Tile) else x for x in reads]
        writes = [x.r if isinstance(x, Tile) else x for x in writes]
        self._dowaits(e, self._deps(reads, writes))
        ins = fn(self.eng[e])
        k = self.own[e]
        self.issued[k] += 1
        ins.then_inc(self.sems[k], 1)
        self._mark((k, self.issued[k]), reads, writes)
        return ins

    def dma(self, q, fn, reads, writes, side):
        reads = [x.r if isinstance(x, Tile) else x for x in reads]
        writes = [x.r if isinstance(x, Tile) else x for x in writes]
        side = side.r if isinstance(side, Tile) else side
        if side.sem is None:
            side.sem = self._newsem("d_" + side.name)
        self._dowaits(q, self._deps(reads, writes))
        ins = fn(self.eng[q])
        k = side.sem
        self.issued[k] += 16
        ins.then_inc(self.sems[k], 16)
        self._mark((k, self.issued[k]), reads, writes)
        return ins

    def barrier(self, engines=None):
        for e in (engines or self.eng):
            deps = {k: v for k, v in enumerate(self.issued) if v > 0}
            self._dowaits(e, deps)


def own_blocks(c, nblk):
    res = []
    for m in range(nblk // 4):
        res += [4 * m, 4 * m + 3] if c == 0 else [4 * m + 1, 4 * m + 2]
    return res


def build(SEQ, dbg=False, phases=(1, 2, 3, 4)):
    NBLK = SEQ // 128
    NSLOT = NBLK // 2
    NT1 = SEQ // 512
    NOWN = NSLOT * 128
    nc = bass.Bass("TRN2", target_bir_lowering=False)
    tk = TK(nc)

    def din(name, shape, dt=F32):
        return nc.dram_tensor(name, list(shape), dt, kind="ExternalInput").ap()

    skind = "ExternalOutput" if dbg else "Internal"

    def dscr(name, shape, dt):
        return nc.dram_tensor(name, list(shape), dt, kind=skind).ap()

    xk = din("xk", [SEQ, D])
    xq = din("xq", [NOWN, D])
    mem = din("mem", [256, D])
    w_in = din("w_in", [D, 3328])
    w_out = din("w_out", [D, D])
    w_kv = din("w_kv", [D, 512])
    w_pq = din("w_pq", [D, 2048])
    keysT = din("keysT", [128, 16, 128])
    peer_u = din("peer_u", [N_EXP, D])
    peer_v = din("peer_v", [N_EXP, D])
    g_mix = din("g_mix", [128, 8])
    g_mem = din("g_mem", [128, 8])
    g_ffn_bc = din("g_ffn_bc", [128, D])
    g_out = din("g_out", [128, 8])
    gam = din("gam", [128, 2, 4])
    g_mq = din("g_mq", [128, 1])
    g_mk = din("g_mk", [128, 1])
    c_ident = din("c_ident", [128, 128])
    c_maskst = din("c_maskst", [128, 128])
    c_reset = din("c_reset", [128, 512])
    c_maskg = din("c_maskg", [128, 2, 256])
    c_blend = din("c_blend", [128, 4])
    c_iota16 = din("c_iota16", [128, 16])
    out = nc.dram_tensor("out", [NOWN, D], F32, kind="ExternalOutput").ap()

    KT_d = dscr("KT_d", [4, 128, SEQ], BF16)
    V_d = dscr("V_d", [NBLK, 128, 512], BF16)
    QT_d = dscr("QT_d", [4, 128, NOWN], BF16)
    MIX_d = dscr("MIX_d", [NOWN, D], BF16)
    R_KT, R_V, R_QT, R_MIX = Reg("KT_d"), Reg("V_d"), Reg("QT_d"), Reg("MIX_d")
    R_OUT = Reg("out")
    UV_d = nc.dram_tensor("UV_d", [N_EXP, 2 * D], BF16, kind="Internal").ap()
    R_UV = Reg("UV_d")

    def mk(es, name, shape, dt=F32, psum=False):
        if psum:
            t = es.enter_context(nc.psum_tensor(name, [128, 512], F32))
            if dt == BF16:
                return Tile(t, name, view=t[:].bitcast(BF16)[:, 0:shape[1]])
            return Tile(t, name, view=t[:][:, 0:shape[1]])
        t = es.enter_context(nc.sbuf_tensor(name, list(shape), dt))
        return Tile(t, name)

    evac_i = [0]

    evac_act_only = [False]

    def evac(out_ap, in_ap, reads, writes, scale=None):
        evac_i[0] += 1
        if evac_i[0] % 2 == 0 or evac_act_only[0]:
            if scale is None:
                tk.op("act", lambda e: e.activation(out=out_ap, in_=in_ap, func=AF.Copy), reads, writes)
            else:
                tk.op("act", lambda e: e.activation(out=out_ap, in_=in_ap, func=AF.Copy, scale=scale), reads, writes)
        else:
            if scale is None:
                tk.op("dve", lambda e: e.tensor_copy(out=out_ap, in_=in_ap), reads, writes)
            else:
                tk.op("dve", lambda e: e.tensor_scalar(out=out_ap, in0=in_ap, scalar1=scale, scalar2=None, op0=ALU.mult), reads, writes)

    with ExitStack() as gs:
        ident_f = mk(gs, "ident_f", [128, 128])
        ident = mk(gs, "ident", [128, 128], BF16)
        maskst = mk(gs, "maskst", [128, 128])
        resetm = mk(gs, "resetm", [128, 512])
        ones = mk(gs, "ones", [128, 512])
        maskg = mk(gs, "maskg", [128, 2, 256])
        blend = mk(gs, "blend", [128, 4])
        iota16 = mk(gs, "iota16", [128, 16])
        gmix = mk(gs, "gmix", [128, 8])
        gmem = mk(gs, "gmem", [128, 8])
        gout = mk(gs, "gout", [128, 8])
        gamt = mk(gs, "gamt", [128, 2, 4])
        lb = mk(gs, "lb", [128, 4])
        oml = mk(gs, "oml", [128, 4])
        gq = mk(gs, "gq", [128, 1])
        gk = mk(gs, "gk", [128, 1])
        qkg = mk(gs, "qkg", [128, 1])
        memKT = mk(gs, "memKT", [128, 2, 256], BF16)
        memV = mk(gs, "memV", [128, 2, 256], BF16)

        for t, src in ((ident_f, c_ident), (maskst, c_maskst), (resetm, c_reset), (maskg, c_maskg),
                       (blend, c_blend), (iota16, c_iota16), (gmix, g_mix), (gmem, g_mem),
                       (gout, g_out), (gamt, gam), (gq, g_mq), (gk, g_mk)):
            tk.dma("sp", lambda e, t=t, src=src: e.dma_start(out=t[:], in_=src), [], [t], t)
        if 4 in phases:
            for (src_, c0_) in ((peer_u, 0), (peer_v, D)):
                for r0 in range(0, N_EXP, 2048):
                    tk.dma("pool", lambda e: e.dma_start(out=UV_d[r0:r0 + 2048, c0_:c0_ + D], in_=src_[r0:r0 + 2048, :]), [], [R_UV], R_UV)
        tk.op("dve", lambda e: e.tensor_copy(out=ident[:], in_=ident_f[:]), [ident_f], [ident])
        tk.op("dve", lambda e: e.memset(ones[:], 1.0), [], [ones])
        tk.op("dve", lambda e: e.tensor_tensor(out=lb[:], in0=gamt[:, 0, :], in1=gamt[:, 1, :], op=ALU.subtract), [gamt], [lb])
        tk.op("act", lambda e: e.activation(out=lb[:], in_=lb[:], func=AF.Sigmoid), [lb], [lb])
        tk.op("dve", lambda e: e.tensor_scalar(out=oml[:], in0=lb[:], scalar1=-1.0, scalar2=1.0, op0=ALU.mult, op1=ALU.add), [lb], [oml])
        tk.op("dve", lambda e: e.tensor_tensor(out=qkg[:], in0=gq[:], in1=gk[:], op=ALU.mult), [gq, gk], [qkg])
        tk.op("dve", lambda e: e.tensor_scalar(out=qkg[:], in0=qkg[:], scalar1=0.125, scalar2=None, op0=ALU.mult), [qkg], [qkg])

        def load_weight(es, dst, src, ranges, gain, tag):
            wmax = max(b - a for a, b in ranges)
            es = ExitStack()
            stg = RR([mk(es, f"wstg{tag}{i}", [128, wmax]) for i in range(2)])
            for c in range(NCH):
                off = 0
                for (a, b) in ranges:
                    n = b - a
                    s = stg.next()
                    tk.dma("sp", lambda e, s=s, c=c, a=a, b=b, n=n: e.dma_start(out=s[:, 0:n], in_=src[c * 128:(c + 1) * 128, a:b]), [], [s], s)
                    if gain is None:
                        tk.op("act", lambda e, s=s, c=c, off=off, n=n: e.activation(out=dst[:, c, off:off + n], in_=s[:, 0:n], func=AF.Copy), [s], [dst])
                    else:
                        tk.op("act", lambda e, s=s, c=c, off=off, n=n: e.activation(out=dst[:, c, off:off + n], in_=s[:, 0:n], func=AF.Copy, scale=gain[:, c:c + 1]), [s, gain], [dst])
                    off += n
            tk.barrier()
            es.close()

        def rmsnorm_rows(xb_ap, xb_reg, junk, ss, rt, out_ap, out_reg, gain_bc=None):
            tk.op("act", lambda e: e.activation(out=junk[:], in_=xb_ap, func=AF.Square, accum_out=ss[:]), [xb_reg], [junk, ss])
            tk.op("act", lambda e: e.activation(out=rt[:], in_=ss[:], func=AF.Sqrt, scale=1.0 / D, bias=EPS), [ss], [rt])
            tk.op("dve", lambda e: e.reciprocal(out=rt[:], in_=rt[:]), [rt], [rt])
            if gain_bc is None:
                tk.op("dve", lambda e: e.tensor_scalar(out=out_ap, in0=xb_ap, scalar1=rt[:, 0:1], scalar2=None, op0=ALU.mult), [xb_reg, rt], [out_reg])
            else:
                tk.op("dve", lambda e: e.scalar_tensor_tensor(out=out_ap, in0=xb_ap, scalar=rt[:, 0:1], in1=gain_bc[:], op0=ALU.mult, op1=ALU.mult), [xb_reg, rt, gain_bc], [out_reg])

        with ExitStack() as es:
            wkv = mk(es, "wkv", [128, NCH, 512], BF16)
            load_weight(es, wkv, w_kv, [(0, 512)], gmem, "kv")
            mx = mk(es, "mx", [128, 2, D])
            mn = mk(es, "mn", [128, 2, D], BF16)
            mnT = mk(es, "mnT", [128, NCH, 256], BF16)
            junk = mk(es, "mjunk", [128, D], BF16)
            ss = mk(es, "mss", [128, 1]); rt = mk(es, "mrt", [128, 1])
            tp = RR([mk(es, f"mtp{i}", [128, 512], BF16, psum=True) for i in range(2)])
            kvps = RR([mk(es, f"mkv{i}", [128, 512], F32, psum=True) for i in range(2)])
            kvs = mk(es, "kvs", [128, 2, 512])
            kn = mk(es, "kn", [128, 2, 256], BF16)
            ss4 = mk(es, "ss4", [128, 4]); j64 = mk(es, "j64", [128, 64])
            tk.dma("sp", lambda e: e.dma_start(out=mx[:], in_=mem.rearrange("(j p) d -> p j d", p=128)), [], [mx], mx)
            for j in range(2):
                rmsnorm_rows(mx[:, j, :], mx, junk, ss, rt, mn[:, j, :], mn)
            for c in range(NCH):
                p = tp.next()
                for j in range(2):
                    tk.op("pe", lambda e, p=p, j=j, c=c: e.transpose(out=p[:, j * 128:(j + 1) * 128], in_=mn[:, j, c * 128:(c + 1) * 128], identity=ident[:]), [mn, ident], [p])
                evac(mnT[:, c, :], p[:, 0:256], [p], [mnT])
            for j in range(2):
                ps = kvps.next()
                for c in range(NCH):
                    tk.op("pe", lambda e, ps=ps, j=j, c=c: e.matmul(ps[:], lhsT=mnT[:, c, j * 128:(j + 1) * 128], rhs=wkv[:, c, :], start=(c == 0), stop=(c == NCH - 1)), [mnT, wkv], [ps])
                evac(kvs[:, j, :], ps[:], [ps], [kvs])
                tk.op("dve", lambda e, j=j: e.tensor_copy(out=memV[:, j, :], in_=kvs[:, j, 256:512]), [kvs], [memV])
                for h in range(4):
                    tk.op("act", lambda e, j=j, h=h: e.activation(out=j64[:], in_=kvs[:, j, h * 64:(h + 1) * 64], func=AF.Square, accum_out=ss4[:, h:h + 1]), [kvs], [j64, ss4])
                tk.op("act", lambda e: e.activation(out=ss4[:], in_=ss4[:], func=AF.Sqrt, scale=1.0 / 64, bias=EPS), [ss4], [ss4])
                tk.op("dve", lambda e: e.reciprocal(out=ss4[:], in_=ss4[:]), [ss4], [ss4])
                for h in range(4):
                    tk.op("dve", lambda e, j=j, h=h: e.tensor_scalar(out=kn[:, j, h * 64:(h + 1) * 64], in0=kvs[:, j, h * 64:(h + 1) * 64], scalar1=ss4[:, h:h + 1], scalar2=None, op0=ALU.mult), [kvs, ss4], [kn])
            for pr in range(2):
                p = tp.next()
                for j in range(2):
                    tk.op("pe", lambda e, p=p, j=j, pr=pr: e.transpose(out=p[:, j * 128:(j + 1) * 128], in_=kn[:, j, pr * 128:(pr + 1) * 128], identity=ident[:]), [kn, ident], [p])
                tk.op("dve", lambda e, p=p, pr=pr: e.tensor_scalar(out=memKT[:, pr, :], in0=p[:, 0:256], scalar1=qkg[:, 0:1], scalar2=None, op0=ALU.mult), [p, qkg], [memKT])
            tk.barrier()

        ss_stack = ExitStack()
        sslot = mk(ss_stack, "sslot", [128, NSLOT, 4, 64], BF16)
        tk.op("pool", lambda e: e.memset(sslot[:], 0.0), [], [sslot])

        if 1 in phases:
          with ExitStack() as es:
            W1 = mk(es, "W1", [128, NCH, 1792], BF16)
            load_weight(es, W1, w_in, [(512, 1536), (2048, 2816)], gmix, "1")
            xb = RR([mk(es, f"xb{i}", [128, 4, D]) for i in range(2)])
            xn = RR([mk(es, f"xn{i}", [128, 4, D], BF16) for i in range(2)])
            hT = RR([mk(es, f"hT{i}", [128, NCH, 512], BF16) for i in range(2)])
            junk = mk(es, "junk1", [128, D], BF16)
            ssr = RR([mk(es, f"ss1_{i}", [128, 1]) for i in range(4)])
            rtr = RR([mk(es, f"rt1_{i}", [128, 1]) for i in range(4)])
            tp = RR([mk(es, f"tp1_{i}", [128, 512], BF16, psum=True) for i in range(2)])
            mm = RR([mk(es, f"mm1_{i}", [128, 512], F32, psum=True) for i in range(4)])
            dsp = RR([mk(es, f"ds1_{i}", [128, 512], F32, psum=True) for i in range(2)])
            ktsb = RR([mk(es, f"ktsb{i}", [128, 512], BF16) for i in range(4)])
            vsb = RR([mk(es, f"vsb{i}", [128, 4, 512], BF16) for i in range(2)])
            vhgA = RR([mk(es, f"vhgA{i}", [128, 4, 256], BF16) for i in range(2)])
            vhgB = RR([mk(es, f"vhgB{i}", [128, 4, 256], BF16) for i in range(2)])
            for t_ in vhgA.tiles + vhgB.tiles:
                tk.op("pool", lambda e, t_=t_: e.memset(t_[:], 0.0), [], [t_])
            sg = RR([mk(es, f"sg{i}", [128, 512]) for i in range(2)])
            fb = RR([mk(es, f"fb{i}", [128, 512]) for i in range(2)])
            gb = RR([mk(es, f"gb{i}", [128, 512]) for i in range(2)])
            bc = RR([mk(es, f"bc{i}", [128, 512]) for i in range(2)])
            em = RR([mk(es, f"em{i}", [128, 512]) for i in range(2)])
            ktl = RR([mk(es, f"ktl{i}", [128, 512], BF16) for i in range(8)])
            ktT = RR([mk(es, f"ktT{i}", [128, 4, 128], BF16) for i in range(8)])
            el = RR([mk(es, f"el{i}", [128, 8]) for i in range(8)])
            S = [mk(es, f"S{h}", [128, 64]) for h in range(4)]
            Stmp = RR([mk(es, f"Stmp{i}", [128, 64]) for i in range(2)])
            for h in range(4):
                tk.op("dve", lambda e, h=h: e.memset(S[h][:], 0.0), [], [S[h]])

            def load_x(T):
                t = xb.next()
                tk.dma("sp", lambda e: e.dma_start(out=t[:], in_=xk[T * 512:(T + 1) * 512, :].rearrange("(j p) d -> p j d", p=128)), [], [t], t)
                return t
            pend = [None]

            def run_b2(T, hp, vh):
                hp2 = []
                for hd in range(4):
                    k_, kT_, el_ = hp[hd]
                    p = tp.next()
                    for j in range(4):
                        tk.op("pe", lambda e, j=j: e.transpose(out=p[:, j * 128:(j + 1) * 128], in_=k_[:, j * 128:(j + 1) * 128], identity=ident[:]), [k_, ident], [p])
                    evac(kT_[:], p[:].rearrange("p (j k) -> p j k", j=4), [p], [kT_])
                    hp2.append((kT_, el_))
                hp = hp2
                for h0 in (0, 2):
                    banks = {}
                    for hd in (h0, h0 + 1):
                        kT_, el_ = hp[hd]
                        dps = dsp.next(); banks[hd] = dps
                        for ch in range(8):
                            j, half = ch // 2, ch % 2
                            tk.op("pe", lambda e, ch=ch, j=j, half=half: e.matmul(dps[:, ch * 64:(ch + 1) * 64], lhsT=kT_[:, j, :], rhs=vh[half][:, j, hd * 64:(hd + 1) * 64], start=True, stop=True), [kT_, vh[half]], [dps])
                    for ch in range(8):
                        j, half = ch // 2, ch % 2
                        for hd in (h0, h0 + 1):
                            kT_, el_ = hp[hd]; dps = banks[hd]
                            tk.op("dve", lambda e, ch=ch: e.scalar_tensor_tensor(out=S[hd][:], in0=S[hd][:], scalar=el_[:, ch:ch + 1], in1=dps[:, ch * 64:(ch + 1) * 64], op0=ALU.mult, op1=ALU.add), [S[hd], el_, dps], [S[hd]])
                            if half == 1:
                                B = T * 4 + j + 1
                                if B < NBLK:
                                    m_, r_ = B // 4, B % 4
                                    sl = 2 * m_ + (0 if r_ < 2 else 1)
                                    tk.op("dve", lambda e, sl=sl, r_=r_: e.scalar_tensor_tensor(out=sslot[:, sl, hd, :], in0=S[hd][:], scalar=blend[:, r_:r_ + 1], in1=sslot[:, sl, hd, :], op0=ALU.mult, op1=ALU.add), [S[hd], blend, sslot], [sslot])


            nxt = load_x(0)
            for T in range(NT1):
                xt = nxt
                if T + 1 < NT1:
                    nxt = load_x(T + 1)
                xnt = xn.next(); h = hT.next()
                for j in range(4):
                    rmsnorm_rows(xt[:, j, :], xt, junk, ssr.next(), rtr.next(), xnt[:, j, :], xnt)
                for c in range(NCH):
                    p = tp.next()
                    for j in range(4):
                        tk.op("pe", lambda e, p=p, j=j, c=c: e.transpose(out=p[:, j * 128:(j + 1) * 128], in_=xnt[:, j, c * 128:(c + 1) * 128], identity=ident[:]), [xnt, ident], [p])
                    evac(h[:, c, :], p[:], [p], [h])
                for pr in range(4):
                    ps = mm.next()
                    for c in range(NCH):
                        tk.op("pe", lambda e, ps=ps, c=c, pr=pr: e.matmul(ps[:], lhsT=W1[:, c, pr * 128:(pr + 1) * 128], rhs=h[:, c, :], start=(c == 0), stop=(c == NCH - 1)), [W1, h], [ps])
                    ks = ktsb.next()
                    evac(ks[:], ps[:], [ps], [ks])
                    tk.dma("sp", lambda e, ks=ks, pr=pr: e.dma_start(out=KT_d[pr, :, T * 512:(T + 1) * 512], in_=ks[:]), [ks], [R_KT], ks)
                vs = vsb.next()
                for j in range(4):
                    ps = mm.next()
                    for c in range(NCH):
                        tk.op("pe", lambda e, ps=ps, c=c, j=j: e.matmul(ps[:], lhsT=h[:, c, j * 128:(j + 1) * 128], rhs=W1[:, c, 512:1024], start=(c == 0), stop=(c == NCH - 1)), [W1, h], [ps])
                    evac(vs[:, j, :], ps[:], [ps], [vs])
                tk.dma("sp", lambda e, vs=vs: e.dma_start(out=V_d[T * 4:(T + 1) * 4].rearrange("j p c -> p j c"), in_=vs[:]), [vs], [R_V], vs)
                vh = (vhgA.next(), vhgB.next())
                for j in range(4):
                    ps = mm.next()
                    for c in range(NCH):
                        tk.op("pe", lambda e, ps=ps, c=c, j=j: e.matmul(ps[:, 0:256], lhsT=h[:, c, j * 128:(j + 1) * 128], rhs=W1[:, c, 1536:1792], start=(c == 0), stop=(c == NCH - 1)), [W1, h], [ps])
                    evac(vh[0][0:64, j, :], ps[0:64, 0:256], [ps], [vh[0]])
                    evac(vh[1][64:128, j, :], ps[64:128, 0:256], [ps], [vh[1]])
                hp = []
                for hd in range(4):
                    ps = mm.next()
                    for c in range(NCH):
                        tk.op("pe", lambda e, ps=ps, c=c, hd=hd: e.matmul(ps[:], lhsT=W1[:, c, 1024 + hd * 128:1024 + (hd + 1) * 128], rhs=h[:, c, :], start=(c == 0), stop=(c == NCH - 1)), [W1, h], [ps])
                    s_ = sg.next(); f_ = fb.next(); g_ = gb.next(); b_ = bc.next(); e_ = em.next(); k_ = ktl.next(); kT_ = ktT.next(); el_ = el.next()
                    tk.op("act", lambda e: e.activation(out=s_[:], in_=ps[:], func=AF.Sigmoid), [ps], [s_])
                    tk.op("dve", lambda e: e.tensor_scalar(out=f_[:], in0=s_[:], scalar1=oml[:, hd:hd + 1], scalar2=lb[:, hd:hd + 1], op0=ALU.mult, op1=ALU.add), [s_, oml, lb], [f_])
                    tk.op("act", lambda e: e.activation(out=g_[:], in_=f_[:], func=AF.Ln), [f_], [g_])
                    tk.op("dve", lambda e: e.tensor_tensor_scan(out=b_[:], data0=resetm[:], data1=g_[:], initial=0.0, op0=ALU.mult, op1=ALU.add), [resetm, g_], [b_])
                    for ch in range(8):
                        tk.op("act", lambda e, ch=ch: e.activation(out=e_[:, ch * 64:(ch + 1) * 64], in_=b_[:, ch * 64:(ch + 1) * 64], func=AF.Exp, scale=-1.0, bias=b_[:, ch * 64 + 63:ch * 64 + 64]), [b_], [e_])
                    tk.op("act", lambda e: e.activation(out=el_[:], in_=b_[:, 63::64], func=AF.Exp), [b_], [el_])
                    tk.op("dve", lambda e: e.tensor_tensor(out=f_[:], in0=f_[:], in1=e_[:], op=ALU.mult), [f_, e_], [f_])
                    tk.op("dve", lambda e: e.tensor_tensor(out=k_[:], in0=e_[:], in1=f_[:], op=ALU.subtract), [f_, e_], [k_])
                    hp.append((k_, kT_, el_))
                pend_now = (T, hp, vh)
                if pend[0] is not None:
                    run_b2(*pend[0])
                pend[0] = pend_now
            run_b2(*pend[0])
            tk.barrier()

        if 2 in phases:
          with ExitStack() as es:
            W2 = mk(es, "W2", [128, NCH, 2304], BF16)
            load_weight(es, W2, w_in, [(0, 512), (1536, 2560), (2560, 3328)], gmix, "2")
            xb = RR([mk(es, f"xb2_{i}", [128, D]) for i in range(2)])
            xn = RR([mk(es, f"xn2_{i}", [128, D], BF16) for i in range(2)])
            hT = RR([mk(es, f"hT2_{i}", [128, NCH, 128], BF16) for i in range(2)])
            junk = mk(es, "junk2", [128, D], BF16)
            ssr = RR([mk(es, f"ss2_{i}", [128, 1]) for i in range(2)])
            rtr = RR([mk(es, f"rt2_{i}", [128, 1]) for i in range(2)])
            tp = RR([mk(es, f"tp2_{i}", [128, 512], BF16, psum=True) for i in range(2)])
            mm = RR([mk(es, f"mm2_{i}", [128, 512], F32, psum=True) for i in range(5)])
            op_ = mk(es, "op2", [128, 512], F32, psum=True)
            qts = RR([mk(es, f"qts{i}", [128, 4, 128], BF16) for i in range(2)])
            mix = RR([mk(es, f"mix2_{i}", [128, 512], BF16) for i in range(2)])
            tok = RR([mk(es, f"tok2_{i}", [128, 768]) for i in range(2)])
            vown = RR([mk(es, f"vown{i}", [128, 256], BF16) for i in range(2)])
            mqT = RR([mk(es, f"mqT{i}", [128, 2, 128], BF16) for i in range(2)])
            t512 = RR([mk(es, f"t512_{i}", [128, 512]) for i in range(8)])
            b512 = RR([mk(es, f"b512_{i}", [128, 512], BF16) for i in range(6)])
            sc_sb = RR([mk(es, f"scsb{i}", [128, 128], BF16) for i in range(3)])
            kcAr = RR([mk(es, f"kcA{i}", [128, 4, 128], BF16) for i in range(2)])
            kcBr = RR([mk(es, f"kcB{i}", [128, 4, 128], BF16) for i in range(2)])
            for t_ in kcAr.tiles + kcBr.tiles:
                tk.op("pool", lambda e, t_=t_: e.memset(t_[:], 0.0), [], [t_])
            pbf = RR([mk(es, f"pbf{i}", [128, 256], BF16) for i in range(2)])
            pT = RR([mk(es, f"pT{i}", [128, 2, 128], BF16) for i in range(2)])
            small = RR([mk(es, f"sm2_{i}", [128, 8]) for i in range(8)])
            j64 = mk(es, "j64b", [128, 64])
            o_sb = RR([mk(es, f"osb{i}", [128, 256]) for i in range(2)])
            sgate = RR([mk(es, f"sgate{i}", [128, 256]) for i in range(2)])

            def load_x(j):
                t = xb.next()
                tk.dma("sp", lambda e: e.dma_start(out=t[:], in_=xq[j * 128:(j + 1) * 128, :]), [], [t], t)
                return t
            nxt = load_x(0)
            for sl in range(NSLOT):
                xt = nxt
                if sl + 1 < NSLOT:
                    nxt = load_x(sl + 1)
                xnt = xn.next(); h = hT.next()
                rmsnorm_rows(xt[:], xt, junk, ssr.next(), rtr.next(), xnt[:], xnt)
                for half in range(2):
                    p = tp.next()
                    for c4 in range(4):
                        c = half * 4 + c4
                        tk.op("pe", lambda e, p=p, c=c, c4=c4: e.transpose(out=p[:, c4 * 128:(c4 + 1) * 128], in_=xnt[:, c * 128:(c + 1) * 128], identity=ident[:]), [xnt, ident], [p])
                    evac(h[:, half * 4:(half + 1) * 4, :], p[:].rearrange("p (c t) -> p c t", c=4), [p], [h])

                def fm_proj(col0, ngrp):
                    ps = mm.next()
                    for g in range(ngrp):
                        for c in range(NCH):
                            tk.op("pe", lambda e, g=g, c=c: e.matmul(ps[:, g * 128:(g + 1) * 128], lhsT=W2[:, c, col0 + g * 128:col0 + (g + 1) * 128], rhs=h[:, c, :], start=(c == 0), stop=(c == NCH - 1)), [W2, h], [ps])
                    return ps
                ps = fm_proj(0, 4)
                qt = qts.next()
                evac(qt[:], ps[:].rearrange("p (g t) -> p g t", g=4), [ps], [qt])
                tk.dma("sp", lambda e, qt=qt: e.dma_start(out=QT_d[:, :, sl * 128:(sl + 1) * 128].rearrange("g p t -> p g t"), in_=qt[:]), [qt], [R_QT], qt)
                tkt = tok.next()
                psA = mm.next(); psB = mm.next()
                for c in range(NCH):
                    tk.op("pe", lambda e, c=c: e.matmul(psA[:], lhsT=h[:, c, :], rhs=W2[:, c, 1536:2048], start=(c == 0), stop=(c == NCH - 1)), [W2, h], [psA])
                for c in range(NCH):
                    tk.op("pe", lambda e, c=c: e.matmul(psB[:, 0:256], lhsT=h[:, c, :], rhs=W2[:, c, 2048:2304], start=(c == 0), stop=(c == NCH - 1)), [W2, h], [psB])
                evac(tkt[:, 0:512], psA[:], [psA], [tkt])
                evac(tkt[:, 512:768], psB[:, 0:256], [psB], [tkt])
                vo = vown.next()
                tk.op("dve", lambda e: e.tensor_copy(out=vo[:], in_=tkt[:, 0:256]), [tkt], [vo])
                sgt = sgate.next()
                tk.op("act", lambda e: e.activation(out=sgt[:], in_=tkt[:, 256:512], func=AF.Silu), [tkt], [sgt])
                psq = fm_proj(512, 4)
                psf = fm_proj(1024, 4)
                sig = t512.next(); f_ = t512.next(); g_ = t512.next(); b_ = t512.next()
                tk.op("act", lambda e: e.activation(out=sig[:], in_=psf[:], func=AF.Sigmoid), [psf], [sig])
                for hd in range(4):
                    tk.op("dve", lambda e, hd=hd: e.tensor_scalar(out=f_[:, hd * 128:(hd + 1) * 128], in0=sig[:, hd * 128:(hd + 1) * 128], scalar1=oml[:, hd:hd + 1], scalar2=lb[:, hd:hd + 1], op0=ALU.mult, op1=ALU.add), [sig, oml, lb], [f_])
                tk.op("act", lambda e: e.activation(out=g_[:], in_=f_[:], func=AF.Ln), [f_], [g_])
                for hd in range(4):
                    tk.op("dve", lambda e, hd=hd: e.tensor_tensor_scan(out=b_[:, hd * 128:(hd + 1) * 128], data0=ones[:, 0:128], data1=g_[:, hd * 128:(hd + 1) * 128], initial=0.0, op0=ALU.mult, op1=ALU.add), [ones, g_], [b_])
                mmid = small.next(); nmid = small.next()
                tk.op("dve", lambda e: e.tensor_copy(out=mmid[:, 0:4], in_=b_[:, 63::128]), [b_], [mmid])
                tk.op("dve", lambda e: e.tensor_scalar(out=nmid[:, 0:4], in0=mmid[:, 0:4], scalar1=-1.0, scalar2=None, op0=ALU.mult), [mmid], [nmid])
                ecp = t512.next(); ecm = t512.next(); ep = t512.next(); sq = t512.next()
                for hd in range(4):
                    sl_ = slice(hd * 128, (hd + 1) * 128)
                    tk.op("act", lambda e, hd=hd, sl_=sl_: e.activation(out=ecp[:, sl_], in_=b_[:, sl_], func=AF.Exp, bias=nmid[:, hd:hd + 1]), [b_, nmid], [ecp])
                    tk.op("act", lambda e, hd=hd, sl_=sl_: e.activation(out=ecm[:, sl_], in_=b_[:, sl_], func=AF.Exp, scale=-1.0, bias=mmid[:, hd:hd + 1]), [b_, mmid], [ecm])
                tk.op("act", lambda e: e.activation(out=ep[:], in_=b_[:], func=AF.Exp), [b_], [ep])
                tk.op("act", lambda e: e.activation(out=sq[:], in_=psq[:], func=AF.Silu), [psq], [sq])
                qc = b512.next(); qh = b512.next()
                tk.op("dve", lambda e: e.tensor_tensor(out=qc[:], in0=sq[:], in1=ecp[:], op=ALU.mult), [sq, ecp], [qc])
                tk.op("dve", lambda e: e.tensor_tensor(out=qh[:], in0=sq[:], in1=ep[:], op=ALU.mult), [sq, ep], [qh])
                tk.op("dve", lambda e: e.tensor_tensor(out=f_[:], in0=f_[:], in1=ecm[:], op=ALU.mult), [f_, ecm], [f_])
                kcA = kcAr.next(); kcB = kcBr.next()
                ecv = ecm[:].rearrange("p (h s) -> p h s", h=4); fv = f_[:].rearrange("p (h s) -> p h s", h=4)
                tk.op("dve", lambda e: e.tensor_tensor(out=kcA[:, :, 0:64], in0=ecv[:, :, 0:64], in1=fv[:, :, 0:64], op=ALU.subtract), [f_, ecm], [kcA])
                tk.op("dve", lambda e: e.tensor_tensor(out=kcB[:, :, 64:128], in0=ecv[:, :, 64:128], in1=fv[:, :, 64:128], op=ALU.subtract), [f_, ecm], [kcB])
                for hd in range(4):
                    sl_ = slice(hd * 128, (hd + 1) * 128)
                    sps = mm.next()
                    tk.op("pe", lambda e, sl_=sl_, sps=sps, hd=hd: e.matmul(sps[:, 0:128], lhsT=kcA[:, hd, :], rhs=qc[:, sl_], start=True, stop=False), [kcA, qc], [sps])
                    tk.op("pe", lambda e, sps=sps, hd=hd: e.matmul(sps[:, 64:128], lhsT=kcB[:, hd, :], rhs=qc[:, hd * 128 + 64:hd * 128 + 128], start=False, stop=True), [kcB, qc], [sps])
                    scs = sc_sb.next()
                    tk.op("dve", lambda e, sps=sps, scs=scs: e.tensor_tensor(out=scs[:], in0=sps[:, 0:128], in1=maskst[:], op=ALU.mult), [sps, maskst], [scs])
                    tk.op("pe", lambda e, scs=scs, hd=hd: e.matmul(op_[:, hd * 64:(hd + 1) * 64], lhsT=scs[:], rhs=vo[:, hd * 64:(hd + 1) * 64], start=True, stop=False), [scs, vo], [op_])
                    tk.op("pe", lambda e, sl_=sl_, hd=hd: e.matmul(op_[:, hd * 64:(hd + 1) * 64], lhsT=qh[:, sl_], rhs=sslot[:, sl, hd, :], start=False, stop=True), [qh, sslot], [op_])
                osb = o_sb.next()
                evac(osb[:], op_[:, 0:256], [op_], [osb])
                ss4 = small.next()
                for hd in range(4):
                    tk.op("act", lambda e, hd=hd: e.activation(out=j64[:], in_=osb[:, hd * 64:(hd + 1) * 64], func=AF.Square, accum_out=ss4[:, hd:hd + 1]), [osb], [j64, ss4])
                tk.op("act", lambda e: e.activation(out=ss4[:, 0:4], in_=ss4[:, 0:4], func=AF.Sqrt, scale=1.0 / 64, bias=EPS), [ss4], [ss4])
                tk.op("dve", lambda e: e.reciprocal(out=ss4[:, 0:4], in_=ss4[:, 0:4]), [ss4], [ss4])
                mx_ = mix.next()
                for hd in range(4):
                    tk.op("dve", lambda e, hd=hd: e.scalar_tensor_tensor(out=mx_[:, hd * 64:(hd + 1) * 64], in0=osb[:, hd * 64:(hd + 1) * 64], scalar=ss4[:, hd:hd + 1], in1=sgt[:, hd * 64:(hd + 1) * 64], op0=ALU.mult, op1=ALU.mult), [osb, ss4, sgt], [mx_])
                psm = fm_proj(2048, 2)
                mq = mqT.next()
                evac(mq[:], psm[:, 0:256].rearrange("p (g t) -> p g t", g=2), [psm], [mq])
                ssq = small.next()
                for hd in range(4):
                    tk.op("act", lambda e, hd=hd: e.activation(out=j64[:], in_=tkt[:, 512 + hd * 64:512 + (hd + 1) * 64], func=AF.Square, accum_out=ssq[:, hd:hd + 1]), [tkt], [j64, ssq])
                tk.op("act", lambda e: e.activation(out=ssq[:, 0:4], in_=ssq[:, 0:4], func=AF.Sqrt, scale=1.0 / 64, bias=EPS), [ssq], [ssq])
                tk.op("dve", lambda e: e.reciprocal(out=ssq[:, 0:4], in_=ssq[:, 0:4]), [ssq], [ssq])
                rs = small.next()
                omp = mm.next()
                for hd in range(4):
                    pr, ph = hd // 2, (hd % 2) * 64
                    lps = mm.next()
                    tk.op("pe", lambda e, lps=lps, pr=pr, ph=ph: e.matmul(lps[:, 0:256], lhsT=mq[ph:ph + 64, pr, :], rhs=memKT[ph:ph + 64, pr, :], start=True, stop=True), [mq, memKT], [lps])
                    pb = pbf.next()
                    tk.op("act", lambda e, lps=lps, pb=pb, hd=hd: e.activation(out=pb[:], in_=lps[:, 0:256], func=AF.Exp, scale=ssq[:, hd:hd + 1], accum_out=rs[:, hd:hd + 1]), [lps, ssq], [pb, rs])
                    p = tp.next()
                    for mb in range(2):
                        tk.op("pe", lambda e, p=p, pb=pb, mb=mb: e.transpose(out=p[:, mb * 128:(mb + 1) * 128], in_=pb[:, mb * 128:(mb + 1) * 128], identity=ident[:]), [pb, ident], [p])
                    pt_ = pT.next()
                    evac(pt_[:], p[:, 0:256].rearrange("p (m t) -> p m t", m=2), [p], [pt_])
                    for mb in range(2):
                        tk.op("pe", lambda e, pt_=pt_, mb=mb, hd=hd: e.matmul(omp[:, hd * 64:(hd + 1) * 64], lhsT=pt_[:, mb, :], rhs=memV[:, mb, hd * 64:(hd + 1) * 64], start=(mb == 0), stop=(mb == 1)), [pt_, memV], [omp])
                tk.op("dve", lambda e: e.reciprocal(out=rs[:, 0:4], in_=rs[:, 0:4]), [rs], [rs])
                for hd in range(4):
                    tk.op("dve", lambda e, hd=hd: e.tensor_scalar(out=mx_[:, 256 + hd * 64:256 + (hd + 1) * 64], in0=omp[:, hd * 64:(hd + 1) * 64], scalar1=rs[:, hd:hd + 1], scalar2=None, op0=ALU.mult), [omp, rs], [mx_])
                tk.dma("sp", lambda e, mx_=mx_: e.dma_start(out=MIX_d[sl * 128:(sl + 1) * 128, 512:1024], in_=mx_[:]), [mx_], [R_MIX], mx_)
            tk.barrier()

        tk.barrier()
        ss_stack.close()

        if 3 in phases:
          with ExitStack() as es:
            KT = RR([mk(es, f"KT{i}", [128, SEQ], BF16) for i in range(2)])
            Vp = RR([mk(es, f"Vp{i}", [128, NBLK, 128], BF16) for i in range(2)])
            QA = RR([mk(es, f"QA{i}", [128, NOWN], BF16) for i in range(2)])
            QB = RR([mk(es, f"QB{i}", [128, NOWN], BF16) for i in range(2)])
            for t_ in QA.tiles + QB.tiles:
                tk.op("pool", lambda e, t_=t_: e.memset(t_[:], 0.0), [], [t_])
            Pu = RR([mk(es, f"Pu{i}", [128, 513]) for i in range(8)])
            zps = RR([mk(es, f"zps{i}", [128, 512], F32, psum=True) for i in range(3)])
            atp = RR([mk(es, f"atp{i}", [128, 512], BF16, psum=True) for i in range(2)])
            ops = RR([mk(es, f"ops{i}", [128, 64], F32, psum=True) for i in range(3)])
            gbuf = RR([mk(es, f"gbuf{i}", [128, 512]) for i in range(6)])
            abuf = RR([mk(es, f"abuf{i}", [128, 512], BF16) for i in range(6)])
            aT = RR([mk(es, f"aT{i}", [128, 512], BF16) for i in range(6)])
            sbo = RR([mk(es, f"sbo{i}", [128, 128], BF16) for i in range(4)])
            ssr = RR([mk(es, f"ss3_{i}", [128, 1]) for i in range(4)])
            j64 = mk(es, "j64c", [128, 64])
            oraw = mk(es, "oraw", [128, NSLOT, 128])
            obf = mk(es, "obf", [128, NSLOT, 128], BF16)
            ssq = mk(es, "ssq3", [128, 2 * NSLOT])
            oraw_regs = [Reg(f"oraw_{i}") for i in range(2 * NSLOT)]
            dcnt = [0]
            negmask = mk(es, "negmask", [128, 2, 256], BF16)
            tk.op("dve", lambda e: e.tensor_scalar(out=negmask[:], in0=maskg[:], scalar1=-30000.0, scalar2=None, op0=ALU.mult), [maskg], [negmask])
            for pr in range(4):
                kt = KT.next(); vp = Vp.next(); qa = QA.next(); qb = QB.next()
                tk.dma("sp", lambda e: e.dma_start(out=kt[:], in_=KT_d[pr]), [R_KT], [kt], kt)
                for jq in range(0, NBLK, 16):
                    j1 = min(NBLK, jq + 16)
                    tk.dma("sp", lambda e: e.dma_start(out=vp[:, jq:j1, :], in_=V_d[jq:j1, :, pr * 128:(pr + 1) * 128].rearrange("j p c -> p j c")), [R_V], [vp], vp)
                tk.dma("sp", lambda e: e.dma_start(out=qa[0:64, :], in_=QT_d[pr, 0:64, :]), [R_QT], [qa], qa)
                tk.dma("sp", lambda e: e.dma_start(out=qb[64:128, :], in_=QT_d[pr, 64:128, :]), [R_QT], [qb], qb)
                qpad = (qa, qb)
                U = []
                for sl in range(NSLOT):
                    m_, par = sl // 2, sl % 2
                    nb = 4 * m_ + (2 if par == 0 else 4)
                    nk = nb * 128
                    units = [(nk - 256, nk, True)]
                    hi = nk - 256
                    while hi > 0:
                        lo = max(0, hi - 512)
                        units.append((lo, hi, False))
                        hi = lo
                    grps = [{"sl": sl, "hh": hh, "nk": nk, "par": par, "nb": nb, "prev": None} for hh in range(2)]
                    bd = 0
                    for ui, (k0, k1, masked) in enumerate(units):
                        for hh in range(2):
                            U.append({"g": grps[hh], "k0": k0, "k1": k1, "m": masked, "first": ui == 0,
                                      "last": ui == len(units) - 1, "bd": bd})
                        bd += (k1 - k0) // 128
                so_by_slot = {}

                def stA(u):
                    g_ = u["g"]; ph = g_["hh"] * 64; sl = g_["sl"]; w = u["k1"] - u["k0"]
                    if u["first"]:
                        g_["o"] = ops.next()
                    z = zps.next()
                    qp = qpad[g_["hh"]]
                    tk.op("pe", lambda e: e.matmul(z[:, 0:w], lhsT=qp[:, sl * 128:(sl + 1) * 128], rhs=kt[:, u["k0"]:u["k1"]], start=True, stop=not u["m"]), [qp, kt], [z])
                    if u["m"]:
                        tk.op("pe", lambda e: e.matmul(z[:, 0:256], lhsT=ident[:], rhs=negmask[:, g_["par"], :], start=False, stop=True), [ident, negmask], [z])
                    g = gbuf.next()
                    tk.op("act", lambda e: e.activation(out=g[:, 0:w], in_=z[:, 0:w], func=AF.Sigmoid, scale=-0.125), [z], [g])
                    u["gb"] = g

                def stB(u):
                    g_ = u["g"]; k0, k1 = u["k0"], u["k1"]; w = k1 - k0; g = u["gb"]
                    P_ = Pu.next(); prev = g_["prev"]
                    if not hasattr(P_, "rc"):
                        P_.rc = Reg(P_.r.name + "_c")
                    if prev is None:
                        tk.op("pool", lambda e: e.memset(P_[:, w:w + 1], 1.0), [], [P_.rc])
                        tk.op("dve", lambda e: e.tensor_tensor_scan(out=P_[:, 0:w][:, ::-1], data0=g[:, 0:w][:, ::-1], data1=ones[:, 0:w], initial=1.0, op0=ALU.mult, op1=ALU.mult), [g, ones], [P_])
                    else:
                        tk.op("act", lambda e: e.activation(out=P_[:, w:w + 1], in_=prev[:, 0:1], func=AF.Copy), [prev], [P_.rc])
                        tk.op("dve", lambda e: e.tensor_tensor_scan(out=P_[:, 0:w][:, ::-1], data0=g[:, 0:w][:, ::-1], data1=ones[:, 0:w], initial=prev[:, 0:1], op0=ALU.mult, op1=ALU.mult), [g, ones, prev], [P_])
                    g_["prev"] = P_
                    a = abuf.next()
                    dcnt[0] += 1
                    de = "pool"
                    tk.op(de, lambda e: e.tensor_tensor(out=a[:, 0:w], in0=P_[:, 1:w + 1], in1=P_[:, 0:w], op=ALU.subtract), [P_, P_.rc], [a])
                    u["a"] = a

                def stC(u):
                    w = u["k1"] - u["k0"]; a = u["a"]
                    tp_ = atp.next()
                    for b in range(w // 128):
                        tk.op("pe", lambda e, b=b: e.transpose(out=tp_[:, b * 128:(b + 1) * 128], in_=a[:, b * 128:(b + 1) * 128], identity=ident[:]), [a, ident], [tp_])
                    at = aT.next()
                    tk.op("act", lambda e: e.activation(out=at[:, 0:w], in_=tp_[:, 0:w], func=AF.Copy), [tp_], [at])
                    u["at"] = at

                def stD(u):
                    g_ = u["g"]; ph = g_["hh"] * 64; sl = g_["sl"]; w = u["k1"] - u["k0"]; at = u["at"]; o_ps = g_["o"]
                    for b in range(w // 128):
                        kb = u["k0"] // 128 + b
                        bd = u["bd"] + b
                        tk.op("pe", lambda e, b=b, kb=kb, bd=bd: e.matmul(o_ps[:], lhsT=at[:, b * 128:(b + 1) * 128], rhs=vp[:, kb, ph:ph + 64], start=(bd == 0), stop=(bd == g_["nb"] - 1)), [at, vp], [o_ps])
                    if u["last"]:
                        hh = g_["hh"]
                        rg = oraw_regs[sl * 2 + hh]
                        tk.op("dve", lambda e: e.tensor_copy(out=oraw[:, sl, ph:ph + 64], in_=o_ps[:]), [o_ps], [rg])
                        tk.op("dve", lambda e: e.scalar_tensor_tensor(out=j64[:], in0=oraw[:, sl, ph:ph + 64], scalar=1.0, in1=oraw[:, sl, ph:ph + 64], op0=ALU.mult, op1=ALU.mult, accum_out=ssq[:, sl * 2 + hh:sl * 2 + hh + 1]), [rg], [j64, rg])

                N = len(U)
                SK = 2
                for i in range(N + 3 * SK):
                    if i < N:
                        stA(U[i])
                    if 0 <= i - SK < N:
                        stB(U[i - SK])
                    if 0 <= i - 2 * SK < N:
                        stC(U[i - 2 * SK])
                    if 0 <= i - 3 * SK < N:
                        stD(U[i - 3 * SK])
                tk.op("act", lambda e: e.activation(out=ssq[:], in_=ssq[:], func=AF.Sqrt, scale=1.0 / 64, bias=EPS), oraw_regs, [ssq])
                tk.op("dve", lambda e: e.reciprocal(out=ssq[:], in_=ssq[:]), [ssq], [ssq])
                tk.op("dve", lambda e: e.tensor_tensor(out=obf[:].rearrange("p s (h d) -> p s h d", h=2), in0=oraw[:].rearrange("p s (h d) -> p s h d", h=2), in1=ssq[:].rearrange("p (s h) -> p s h", h=2).unsqueeze(3).broadcast_to([128, NSLOT, 2, 64]), op=ALU.mult), oraw_regs + [ssq], [obf])
                tk.dma("sp", lambda e: e.dma_start(out=MIX_d[:, pr * 128:(pr + 1) * 128].rearrange("(s p) c -> p s c", p=128), in_=obf[:]), [obf], [R_MIX], obf)
            tk.barrier()

        if 4 in phases:
          with ExitStack() as es:
            NG = 11
            Wo = mk(es, "Wo", [128, NCH, D], BF16)
            load_weight(es, Wo, w_out, [(0, 512), (512, 1024)], gout, "o")
            Wq = mk(es, "Wq", [128, NCH, 2048], BF16)
            load_weight(es, Wq, w_pq, [(0, 1024), (1024, 2048)], None, "q")
            kT = mk(es, "kT", [128, 16, 128], BF16)
            with ExitStack() as es2:
                kstg = mk(es2, "kstg", [128, 16, 128])
                tk.dma("sp", lambda e: e.dma_start(out=kstg[:], in_=keysT), [], [kstg], kstg)
                tk.op("dve", lambda e: e.tensor_copy(out=kT[:], in_=kstg[:]), [kstg], [kT])
                tk.barrier()
            gffn = mk(es, "gffn", [128, D])
            tk.dma("sp", lambda e: e.dma_start(out=gffn[:], in_=g_ffn_bc), [], [gffn], gffn)
            xb = RR([mk(es, f"xb4_{i}", [128, D]) for i in range(2)])
            mxb = RR([mk(es, f"mxb{i}", [128, D], BF16) for i in range(2)])
            mT = mk(es, "mT", [128, NCH, 128], BF16)
            x1 = RR([mk(es, f"x1_{i}", [128, D]) for i in range(2)])
            h2 = RR([mk(es, f"h2_{i}", [128, D], BF16) for i in range(2)])
            h2T = mk(es, "h2T", [128, NCH, 128], BF16)
            junk = mk(es, "junk4", [128, D], BF16)
            ssr = RR([mk(es, f"ss4_{i}", [128, 1]) for i in range(2)])
            rtr = RR([mk(es, f"rt4_{i}", [128, 1]) for i in range(2)])
            yps = [mk(es, f"yps{i}", [128, 512], F32, psum=True) for i in range(2)]
            tps = mk(es, "tps4", [128, 1024], BF16, psum=True)
            wps = [mk(es, f"wps{i}", [128, 512], F32, psum=True) for i in range(2)]
            qsp = RR([mk(es, f"qsp{i}", [128, 512], F32, psum=True) for i in range(3)])
            qg = mk(es, "qg", [128, 16, 128], BF16)
            sc = mk(es, "sc", [128, 16, 128])
            sc2 = mk(es, "sc2", [128, 16, 128])
            tops = mk(es, "tops", [128, 16, 16])
            topi = mk(es, "topi", [128, 16, 16], U32)
            topf = mk(es, "topf", [128, 16, 16])
            cand = mk(es, "cand", [128, 8, 256])
            cand2 = mk(es, "cand2", [128, 8, 256])
            best = mk(es, "best", [128, 8, 16])
            pos = mk(es, "pos", [128, 8, 16], U32)
            posa = mk(es, "posa", [128, 8, 16], U32)
            posb = mk(es, "posb", [128, 8, 16], U32)
            paf = mk(es, "paf", [128, 8, 16])
            pbf_ = mk(es, "pbf_", [128, 8, 16])
            eq = mk(es, "eq", [128, 8, 16, 16])
            i1s = mk(es, "i1s", [128, 8, 16])
            i2s = mk(es, "i2s", [128, 8, 16])
            eidf = mk(es, "eidf", [128, 128])
            eidxr = RR([mk(es, f"eidx{i}", [128, 128], U32) for i in range(2)])
            gater = RR([mk(es, f"gate{i}", [128, 8, 16]) for i in range(2)])
            gsum = mk(es, "gsum", [128, 8])
            actr = [mk(es, f"actr{i}", [128, 128]) for i in range(2)]
            glr = [mk(es, f"glr{i}", [128, 128]) for i in range(2)]
            actr_regs = [[Reg(f"actr{i}_{s}") for s in range(128)] for i in range(2)]
            glr_regs = [[Reg(f"glr{i}_{s}") for s in range(128)] for i in range(2)]
            wr = [mk(es, f"wr{i}", [128, 128]) for i in range(2)]
            wr_regs = [[Reg(f"wr{i}_{s}") for s in range(128)] for i in range(2)]
            uvb = RR([mk(es, f"uvb{i}", [128, 2 * D], BF16) for i in range(NG)])
            dg = RR([mk(es, f"dg{i}", [128, 128], BF16) for i in range(6)])
            ttj = mk(es, "ttj", [128, D], BF16)
            prodb = RR([mk(es, f"prodb{i}", [128, D], BF16) for i in range(3)])
            junkA = mk(es, "junkA", [128, D], BF16)

            def load4(sl):
                t = xb.next(); m = mxb.next()
                tk.dma("sp", lambda e: e.dma_start(out=t[:], in_=xq[sl * 128:(sl + 1) * 128, :]), [], [t], t)
                tk.dma("sp", lambda e: e.dma_start(out=m[:], in_=MIX_d[sl * 128:(sl + 1) * 128, :]), [R_MIX], [m], m)
                return t, m

            def front(sl, st):
                xt, mt = load4(sl)
                yield
                for c in range(NCH):
                    tk.op("pe", lambda e, c=c: e.transpose(out=tps[:, c * 128:(c + 1) * 128], in_=mt[:, c * 128:(c + 1) * 128], identity=ident[:]), [mt, ident], [tps])
                evac(mT[:], tps[:].rearrange("p (c t) -> p c t", c=NCH), [tps], [mT])
                for n in range(2):
                    for c in range(NCH):
                        tk.op("pe", lambda e, n=n, c=c: e.matmul(wps[n][:], lhsT=mT[:, c, :], rhs=Wo[:, c, n * 512:(n + 1) * 512], start=(c == 0), stop=(c == NCH - 1)), [mT, Wo], [wps[n]])
                x1_ = x1.next()
                for n in range(2):
                    tk.op("dve", lambda e, n=n: e.tensor_tensor(out=x1_[:, n * 512:(n + 1) * 512], in0=wps[n][:], in1=xt[:, n * 512:(n + 1) * 512], op=ALU.add), [wps[n], xt], [x1_])
                yield
                h2_ = h2.next()
                rmsnorm_rows(x1_[:], x1_, junk, ssr.next(), rtr.next(), h2_[:], h2_, gain_bc=gffn)
                st["x1"] = x1_; st["h2"] = h2_
                yield
                for c in range(NCH):
                    tk.op("pe", lambda e, c=c: e.transpose(out=tps[:, c * 128:(c + 1) * 128], in_=h2_[:, c * 128:(c + 1) * 128], identity=ident[:]), [h2_, ident], [tps])
                evac(h2T[:], tps[:].rearrange("p (c t) -> p c t", c=NCH), [tps], [h2T])
                for q4 in range(4):
                    p = qsp.next()
                    for gi in range(4):
                        g = q4 * 4 + gi
                        for c in range(NCH):
                            tk.op("pe", lambda e, g=g, gi=gi, c=c: e.matmul(p[:, gi * 128:(gi + 1) * 128], lhsT=Wq[:, c, g * 128:(g + 1) * 128], rhs=h2T[:, c, :], start=(c == 0), stop=(c == NCH - 1)), [Wq, h2T], [p])
                    evac(qg[:, q4 * 4:(q4 + 1) * 4, :], p[:].rearrange("p (g t) -> p g t", g=4), [p], [qg])
                    yield
                for q4 in range(4):
                    p = qsp.next()
                    for gi in range(4):
                        g = q4 * 4 + gi
                        tk.op("pe", lambda e, g=g, gi=gi: e.matmul(p[:, gi * 128:(gi + 1) * 128], lhsT=qg[:, g, :], rhs=kT[:, g, :], start=True, stop=True), [qg, kT], [p])
                    evac(sc[:, q4 * 4:(q4 + 1) * 4, :], p[:].rearrange("p (g n) -> p g n", g=4), [p], [sc])
                    yield
                for g in range(16):
                    tk.op("dve", lambda e: e.max(out=tops[:, g, 0:8], in_=sc[:, g, :]), [sc], [tops])
                    tk.op("dve", lambda e: e.max_index(out=topi[:, g, 0:8], in_max=tops[:, g, 0:8], in_values=sc[:, g, :]), [sc, tops], [topi])
                    tk.op("dve", lambda e: e.match_replace(out=sc2[:, g, :], in_to_replace=tops[:, g, 0:8], in_values=sc[:, g, :], imm_value=NEG), [sc, tops], [sc2])
                    tk.op("dve", lambda e: e.max(out=tops[:, g, 8:16], in_=sc2[:, g, :]), [sc2], [tops])
                    tk.op("dve", lambda e: e.max_index(out=topi[:, g, 8:16], in_max=tops[:, g, 8:16], in_values=sc2[:, g, :]), [sc2, tops], [topi])
                    if g % 2 == 1:
                        yield
                tk.op("dve", lambda e: e.tensor_copy(out=topf[:], in_=topi[:]), [topi], [topf])
                tv = tops[:].rearrange("p (h c) k -> p h c k", c=2)
                tfv = topf[:].rearrange("p (h c) k -> p h c k", c=2)
                cv = cand[:].rearrange("p h (a b) -> p h a b", a=16)
                tk.op("dve", lambda e: e.tensor_tensor(out=cv, in0=tv[:, :, 0, :].unsqueeze(3).broadcast_to([128, 8, 16, 16]), in1=tv[:, :, 1, :].unsqueeze(2).broadcast_to([128, 8, 16, 16]), op=ALU.add), [tops], [cand])
                yield
                for hh in range(8):
                    tk.op("dve", lambda e: e.max(out=best[:, hh, 0:8], in_=cand[:, hh, :]), [cand], [best])
                    tk.op("dve", lambda e: e.max_index(out=pos[:, hh, 0:8], in_max=best[:, hh, 0:8], in_values=cand[:, hh, :]), [cand, best], [pos])
                    tk.op("dve", lambda e: e.match_replace(out=cand2[:, hh, :], in_to_replace=best[:, hh, 0:8], in_values=cand[:, hh, :], imm_value=NEG), [cand, best], [cand2])
                    tk.op("dve", lambda e: e.max(out=best[:, hh, 8:16], in_=cand2[:, hh, :]), [cand2], [best])
                    tk.op("dve", lambda e: e.max_index(out=pos[:, hh, 8:16], in_max=best[:, hh, 8:16], in_values=cand2[:, hh, :]), [cand2, best], [pos])
                    if hh % 2 == 1:
                        yield
                tk.op("dve", lambda e: e.tensor_single_scalar(out=posa[:], in_=pos[:], scalar=4, op=ALU.logical_shift_right), [pos], [posa])
                tk.op("dve", lambda e: e.tensor_single_scalar(out=posb[:], in_=pos[:], scalar=15, op=ALU.bitwise_and), [pos], [posb])
                tk.op("dve", lambda e: e.tensor_copy(out=paf[:], in_=posa[:]), [posa], [paf])
                tk.op("dve", lambda e: e.tensor_copy(out=pbf_[:], in_=posb[:]), [posb], [pbf_])
                yield
                io = iota16[:].unsqueeze(1).unsqueeze(1).broadcast_to([128, 8, 16, 16])
                for (pf, cc, dst) in ((paf, 0, i1s), (pbf_, 1, i2s)):
                    tk.op("dve", lambda e: e.tensor_tensor(out=eq[:], in0=io, in1=pf[:].unsqueeze(3).broadcast_to([128, 8, 16, 16]), op=ALU.is_equal), [iota16, pf], [eq])
                    yield
                    tk.op("dve", lambda e: e.tensor_tensor(out=eq[:], in0=eq[:], in1=tfv[:, :, cc, :].unsqueeze(2).broadcast_to([128, 8, 16, 16]), op=ALU.mult), [eq, topf], [eq])
                    yield
                    tk.op("dve", lambda e: e.tensor_reduce(out=dst[:], in_=eq[:], axis=AX.X, op=ALU.add), [eq], [dst])
                    yield
                eidx = eidxr.next(); gate = gater.next()
                tk.op("dve", lambda e: e.scalar_tensor_tensor(out=eidf[:], in0=i1s[:].rearrange("p h k -> p (h k)"), scalar=128.0, in1=i2s[:].rearrange("p h k -> p (h k)"), op0=ALU.mult, op1=ALU.add), [i1s, i2s], [eidf])
                tk.op("dve", lambda e: e.tensor_copy(out=eidx[:], in_=eidf[:]), [eidf], [eidx])
                tk.op("dve", lambda e: e.tensor_tensor(out=gate[:], in0=best[:], in1=best[:, :, 0:1].broadcast_to([128, 8, 16]), op=ALU.subtract), [best], [gate])
                tk.op("act", lambda e: e.activation(out=gate[:], in_=gate[:], func=AF.Exp), [gate], [gate])
                tk.op("dve", lambda e: e.tensor_reduce(out=gsum[:], in_=gate[:], axis=AX.X, op=ALU.add), [gate], [gsum])
                tk.op("dve", lambda e: e.reciprocal(out=gsum[:], in_=gsum[:]), [gsum], [gsum])
                tk.op("dve", lambda e: e.tensor_tensor(out=gate[:], in0=gate[:], in1=gsum[:].unsqueeze(2).broadcast_to([128, 8, 16]), op=ALU.mult), [gate, gsum], [gate])
                st["eidx"] = eidx; st["gate"] = gate

            def back(sl, st, gen):
                eidx = st["eidx"]; gate = st["gate"]; h2_ = st["h2"]; x1_ = st["x1"]
                ab = sl % 2
                gflat = gate[:].rearrange("p h k -> p (h k)")
                LAG = 3
                uvs = {}
                for s in range(128 + LAG):
                    if s < 128:
                        uv = uvb.next(); uvs[s] = uv
                        tk.dma("pool", lambda e: e.indirect_dma_start(out=uv[:], out_offset=None, in_=UV_d, in_offset=bass.IndirectOffsetOnAxis(ap=eidx[:, s:s + 1], axis=0)), [eidx, R_UV], [uv], uv)
                        pr_ = prodb.next()
                        tk.op("dve", lambda e: e.tensor_tensor(out=pr_[:], in0=uv[:, 0:D], in1=h2_[:], op=ALU.mult), [uv, h2_], [pr_])
                        tk.op("act", lambda e: e.activation(out=junkA[:], in_=pr_[:], func=AF.Copy, accum_out=actr[ab][:, s:s + 1]), [pr_], [actr_regs[ab][s]])
                        tk.op("act", lambda e: e.activation(out=glr[ab][:, s:s + 1], in_=actr[ab][:, s:s + 1], func=AF.Gelu), [actr_regs[ab][s]], [glr_regs[ab][s]])
                    r = s - LAG
                    if r >= 0:
                        uvr = uvs.pop(r)
                        d_ = dg.next()
                        tk.op("dve", lambda e: e.tensor_scalar(out=d_[:], in0=ident[:], scalar1=glr[ab][:, r:r + 1], scalar2=gflat[:, r:r + 1], op0=ALU.mult, op1=ALU.mult), [ident, glr_regs[ab][r], gate], [d_])
                        for n in range(2):
                            tk.op("pe", lambda e, n=n: e.matmul(yps[n][:], lhsT=d_[:], rhs=uvr[:, D + n * 512:D + (n + 1) * 512], start=(r == 0), stop=(r == 127)), [d_, uvr], [yps[n]])
                    if gen is not None and s % 4 == 3:
                        next(gen, None)
                if gen is not None:
                    for _ in gen:
                        pass
                for n in range(2):
                    tk.op("dve", lambda e, n=n: e.tensor_tensor(out=x1_[:, n * 512:(n + 1) * 512], in0=yps[n][:], in1=x1_[:, n * 512:(n + 1) * 512], op=ALU.add), [yps[n], x1_], [x1_])
                tk.dma("sp", lambda e: e.dma_start(out=out[sl * 128:(sl + 1) * 128, :], in_=x1_[:]), [x1_], [R_OUT], x1_)

            states = [dict() for _ in range(NSLOT)]
            evac_act_only[0] = True
            for _ in front(0, states[0]):
                pass
            for sl in range(NSLOT):
                gen = front(sl + 1, states[sl + 1]) if sl + 1 < NSLOT else None
                back(sl, states[sl], gen)
            tk.barrier()

        tk.barrier()
    return nc


def host_inputs(inputs, SEQ, cores):
    x = np.asarray(inputs["x"], np.float32)
    NBLK = SEQ // 128
    f32 = np.float32

    def col8(v):
        return np.ascontiguousarray(np.asarray(v, f32).reshape(8, 128).T)
    g_out = np.concatenate([np.asarray(inputs["sb_out_gain"], f32).reshape(-1),
                            np.asarray(inputs["hg_out_gain"], f32).reshape(-1),
                            np.ones(256, f32)])
    gam = np.asarray(inputs["gamma_lb"], f32).reshape(2, 4, 128).transpose(2, 0, 1)
    keys = np.asarray(inputs["peer_sub_keys"], f32).reshape(16, 128, 128)
    shared = {
        "w_in": np.ascontiguousarray(np.asarray(inputs["w_in"], f32)[0]),
        "w_out": np.ascontiguousarray(np.asarray(inputs["w_out"], f32)[0]),
        "w_kv": np.ascontiguousarray(np.asarray(inputs["w_mem_kv"], f32)[0]),
        "w_pq": np.ascontiguousarray(np.asarray(inputs["peer_w_query"], f32)[0]),
        "keysT": np.ascontiguousarray(keys.transpose(2, 0, 1)),
        "peer_u": np.ascontiguousarray(np.asarray(inputs["peer_u"], f32)[0]),
        "peer_v": np.ascontiguousarray(np.asarray(inputs["peer_v"], f32)[0]),
        "g_mix": col8(np.asarray(inputs["norm_mix_gain"])[0]),
        "g_mem": col8(inputs["mem_norm_gain"]),
        "g_ffn_bc": np.ascontiguousarray(np.broadcast_to(np.asarray(inputs["norm_ffn_gain"], f32)[0][None, :], (128, D))),
        "g_out": col8(g_out),
        "gam": np.ascontiguousarray(gam),
        "g_mq": np.ascontiguousarray(np.tile(np.asarray(inputs["mem_q_gain"], f32)[0], 2).reshape(128, 1)),
        "g_mk": np.ascontiguousarray(np.tile(np.asarray(inputs["mem_k_gain"], f32)[0], 2).reshape(128, 1)),
        "c_ident": np.eye(128, dtype=f32),
        "c_maskst": np.triu(np.ones((128, 128), f32)),
        "c_reset": np.ascontiguousarray(np.broadcast_to((np.arange(512) % 64 != 0).astype(f32)[None, :], (128, 512))),
        "c_iota16": np.ascontiguousarray(np.broadcast_to(np.arange(16, dtype=f32)[None, :], (128, 16))),
    }
    tri = (np.arange(128)[None, :] >= np.arange(128)[:, None]).astype(f32)
    allm = np.ones((128, 128), f32)
    none = np.zeros((128, 128), f32)
    A = np.concatenate([tri, allm], axis=1)
    Bm = np.concatenate([none, tri], axis=1)
    maps = []
    for core in cores:
        b, c = core // 2, core % 2
        blocks = own_blocks(c, NBLK)
        xb = x[b, :SEQ].reshape(NBLK, 128, D)
        m = dict(shared)
        m["xk"] = np.ascontiguousarray(x[b, :SEQ])
        m["xq"] = np.ascontiguousarray(xb[blocks].reshape(-1, D))
        m["mem"] = np.ascontiguousarray(np.asarray(inputs["mem"], f32)[b])
        m["c_maskg"] = np.ascontiguousarray(np.stack([A, Bm] if c == 0 else [Bm, A], axis=1))
        bl = np.array([1, 0, 0, 1] if c == 0 else [0, 1, 1, 0], f32)
        m["c_blend"] = np.ascontiguousarray(np.broadcast_to(bl[None, :], (128, 4)))
        maps.append(m)
    return maps


_NC_CACHE = {}


def kernel(**inputs):
    x = np.asarray(inputs["x"])
    B, SEQ, _ = x.shape
    cores = list(range(2 * B))
    if SEQ not in _NC_CACHE:
        _NC_CACHE[SEQ] = build(SEQ)
    nc = _NC_CACHE[SEQ]
    maps = host_inputs(inputs, SEQ, cores)
    res = run_bass_kernel_spmd(nc, maps, core_ids=cores)
    NBLK = SEQ // 128
    outp = np.zeros((B, SEQ, D), np.float32)
    ov = outp.reshape(B, NBLK, 128, D)
    for core in cores:
        b, c = core // 2, core % 2
        blocks = own_blocks(c, NBLK)
        ov[b, blocks] = np.asarray(res.results[core]["out"], np.float32).reshape(len(blocks), 128, D)
    return outp
```

```python
import numpy as np
from contextlib import ExitStack
import concourse.bass as bass
import concourse.mybir as mybir
from concourse.bass_utils import run_bass_kernel_spmd

F32 = mybir.dt.float32
BF16 = mybir.dt.bfloat16
U32 = mybir.dt.uint32
I32 = mybir.dt.int32
ALU = mybir.AluOpType
AF = mybir.ActivationFunctionType
AX = mybir.AxisListType

D = 1024
NCH = 8
EPS = 1e-6
N_EXP = 16384
NEG = -1.0e30


class Reg:
    __slots__ = ("name", "w", "r", "sem", "cnt")

    def __init__(self, name):
        self.name = name
        self.w = None
        self.r = {}
        self.sem = None
        self.cnt = 0


class Tile:
    def __init__(self, t, name, view=None):
        self.t = t
        self.v = view
        self.r = Reg(name)

    def __getitem__(self, k):
        if self.v is not None:
            return self.v[k]
        return self.t[k]


class RR:
    def __init__(self, tiles):
        self.tiles = tiles
        self.i = 0

    def next(self):
        t = self.tiles[self.i % len(self.tiles)]
        self.i += 1
        return t


class TK:
    def __init__(self, nc):
        self.nc = nc
        self.eng = {"pe": nc.tensor, "act": nc.scalar, "dve": nc.vector,
                    "pool": nc.gpsimd, "sp": nc.sync}
        self.sems = []
        self.issued = []
        self.own = {}
        for k in ("pe", "act", "dve", "pool"):
            self.own[k] = self._newsem("s_" + k)
        self.waited = {k: {} for k in self.eng}

    def _newsem(self, name):
        self.sems.append(self.nc.alloc_semaphore(name))
        self.issued.append(0)
        return len(self.sems) - 1

    def _deps(self, reads, writes):
        deps = {}

        def add(k, v):
            if deps.get(k, 0) < v:
                deps[k] = v
        for t in reads:
            if t.w is not None:
                add(*t.w)
        for t in writes:
            if t.w is not None:
                add(*t.w)
            for k, v in t.r.items():
                add(k, v)
        return deps

    def _dowaits(self, e, deps):
        own = self.own.get(e)
        for k, v in deps.items():
            if k == own and e == "pe":
                continue
            if self.waited[e].get(k, 0) >= v:
                continue
            self.eng[e].wait_ge(self.sems[k], v)
            self.waited[e][k] = v

    def _mark(self, tok, reads, writes):
        k, v = tok
        for t in reads:
            if t.r.get(k, 0) < v:
                t.r[k] = v
        for t in writes:
            t.w = tok
            t.r = {}

    def op(self, e, fn, reads=(), writes=()):
        reads = [x.r if isinstance(x, Tile) else x for x in reads]
        writes = [x.r if isinstance(x, Tile) else x for x in writes]
        self._dowaits(e, self._deps(reads, writes))
        ins = fn(self.eng[e])
        k = self.own[e]
        self.issued[k] += 1
        ins.then_inc(self.sems[k], 1)
        self._mark((k, self.issued[k]), reads, writes)
        return ins

    def dma(self, q, fn, reads, writes, side):
        reads = [x.r if isinstance(x, Tile) else x for x in reads]
        writes = [x.r if isinstance(x, Tile) else x for x in writes]
        side = side.r if isinstance(side, Tile) else side
        if side.sem is None:
            side.sem = self._newsem("d_" + side.name)
        self._dowaits(q, self._deps(reads, writes))
        ins = fn(self.eng[q])
        k = side.sem
        self.issued[k] += 16
        ins.then_inc(self.sems[k], 16)
        self._mark((k, self.issued[k]), reads, writes)
        return ins

    def barrier(self, engines=None):
        for e in (engines or self.eng):
            deps = {k: v for k, v in enumerate(self.issued) if v > 0}
            self._dowaits(e, deps)


def own_blocks(c, nblk):
    res = []
    for m in range(nblk // 4):
        res += [4 * m, 4 * m + 3] if c == 0 else [4 * m + 1, 4 * m + 2]
    return res


def build(SEQ, dbg=False, phases=(1, 2, 3, 4)):
    NBLK = SEQ // 128
    NSLOT = NBLK // 2
    NT1 = SEQ // 512
    NOWN = NSLOT * 128
    nc = bass.Bass("TRN2", target_bir_lowering=False)
    tk = TK(nc)

    def din(name, shape, dt=F32):
        return nc.dram_tensor(name, list(shape), dt, kind="ExternalInput").ap()

    skind = "ExternalOutput" if dbg else "Internal"

    def dscr(name, shape, dt):
        return nc.dram_tensor(name, list(shape), dt, kind=skind).ap()

    xk = din("xk", [SEQ, D])
    xq = din("xq", [NOWN, D])
    mem = din("mem", [256, D])
    w_in = din("w_in", [D, 3328])
    w_out = din("w_out", [D, D])
    w_kv = din("w_kv", [D, 512])
    w_pq = din("w_pq", [D, 2048])
    keysT = din("keysT", [128, 16, 128])
    peer_u = din("peer_u", [N_EXP, D])
    peer_v = din("peer_v", [N_EXP, D])
    g_mix = din("g_mix", [128, 8])
    g_mem = din("g_mem", [128, 8])
    g_ffn_bc = din("g_ffn_bc", [128, D])
    g_out = din("g_out", [128, 8])
    gam = din("gam", [128, 2, 4])
    g_mq = din("g_mq", [128, 1])
    g_mk = din("g_mk", [128, 1])
    c_ident = din("c_ident", [128, 128])
    c_maskst = din("c_maskst", [128, 128])
    c_reset = din("c_reset", [128, 512])
    c_maskg = din("c_maskg", [128, 2, 256])
    c_blend = din("c_blend", [128, 4])
    c_iota16 = din("c_iota16", [128, 16])
    out = nc.dram_tensor("out", [NOWN, D], F32, kind="ExternalOutput").ap()

    KT_d = dscr("KT_d", [4, 128, SEQ], BF16)
    V_d = dscr("V_d", [NBLK, 128, 512], BF16)
    QT_d = dscr("QT_d", [4, 128, NOWN], BF16)
    MIX_d = dscr("MIX_d", [NOWN, D], BF16)
    R_KT, R_V, R_QT, R_MIX = Reg("KT_d"), Reg("V_d"), Reg("QT_d"), Reg("MIX_d")
    R_OUT = Reg("out")
    UV_d = nc.dram_tensor("UV_d", [N_EXP, 2 * D], BF16, kind="Internal").ap()
    R_UV = Reg("UV_d")

    def mk(es, name, shape, dt=F32, psum=False):
        if psum:
            t = es.enter_context(nc.psum_tensor(name, [128, 512], F32))
            if dt == BF16:
                return Tile(t, name, view=t[:].bitcast(BF16)[:, 0:shape[1]])
            return Tile(t, name, view=t[:][:, 0:shape[1]])
        t = es.enter_context(nc.sbuf_tensor(name, list(shape), dt))
        return Tile(t, name)

    evac_i = [0]

    evac_act_only = [False]

    def evac(out_ap, in_ap, reads, writes, scale=None):
        evac_i[0] += 1
        if evac_i[0] % 2 == 0 or evac_act_only[0]:
            if scale is None:
                tk.op("act", lambda e: e.activation(out=out_ap, in_=in_ap, func=AF.Copy), reads, writes)
            else:
                tk.op("act", lambda e: e.activation(out=out_ap, in_=in_ap, func=AF.Copy, scale=scale), reads, writes)
        else:
            if scale is None:
                tk.op("dve", lambda e: e.tensor_copy(out=out_ap, in_=in_ap), reads, writes)
            else:
                tk.op("dve", lambda e: e.tensor_scalar(out=out_ap, in0=in_ap, scalar1=scale, scalar2=None, op0=ALU.mult), reads, writes)

    with ExitStack() as gs:
        ident_f = mk(gs, "ident_f", [128, 128])
        ident = mk(gs, "ident", [128, 128], BF16)
        maskst = mk(gs, "maskst", [128, 128])
        resetm = mk(gs, "resetm", [128, 512])
        ones = mk(gs, "ones", [128, 512])
        maskg = mk(gs, "maskg", [128, 2, 256])
        blend = mk(gs, "blend", [128, 4])
        iota16 = mk(gs, "iota16", [128, 16])
        gmix = mk(gs, "gmix", [128, 8])
        gmem = mk(gs, "gmem", [128, 8])
        gout = mk(gs, "gout", [128, 8])
        gamt = mk(gs, "gamt", [128, 2, 4])
        lb = mk(gs, "lb", [128, 4])
        oml = mk(gs, "oml", [128, 4])
        gq = mk(gs, "gq", [128, 1])
        gk = mk(gs, "gk", [128, 1])
        qkg = mk(gs, "qkg", [128, 1])
        memKT = mk(gs, "memKT", [128, 2, 256], BF16)
        memV = mk(gs, "memV", [128, 2, 256], BF16)

        for t, src in ((ident_f, c_ident), (maskst, c_maskst), (resetm, c_reset), (maskg, c_maskg),
                       (blend, c_blend), (iota16, c_iota16), (gmix, g_mix), (gmem, g_mem),
                       (gout, g_out), (gamt, gam), (gq, g_mq), (gk, g_mk)):
            tk.dma("sp", lambda e, t=t, src=src: e.dma_start(out=t[:], in_=src), [], [t], t)
        if 4 in phases:
            for (src_, c0_) in ((peer_u, 0), (peer_v, D)):
                for r0 in range(0, N_EXP, 2048):
                    tk.dma("pool", lambda e: e.dma_start(out=UV_d[r0:r0 + 2048, c0_:c0_ + D], in_=src_[r0:r0 + 2048, :]), [], [R_UV], R_UV)
        tk.op("dve", lambda e: e.tensor_copy(out=ident[:], in_=ident_f[:]), [ident_f], [ident])
        tk.op("dve", lambda e: e.memset(ones[:], 1.0), [], [ones])
        tk.op("dve", lambda e: e.tensor_tensor(out=lb[:], in0=gamt[:, 0, :], in1=gamt[:, 1, :], op=ALU.subtract), [gamt], [lb])
        tk.op("act", lambda e: e.activation(out=lb[:], in_=lb[:], func=AF.Sigmoid), [lb], [lb])
        tk.op("dve", lambda e: e.tensor_scalar(out=oml[:], in0=lb[:], scalar1=-1.0, scalar2=1.0, op0=ALU.mult, op1=ALU.add), [lb], [oml])
        tk.op("dve", lambda e: e.tensor_tensor(out=qkg[:], in0=gq[:], in1=gk[:], op=ALU.mult), [gq, gk], [qkg])
        tk.op("dve", lambda e: e.tensor_scalar(out=qkg[:], in0=qkg[:], scalar1=0.125, scalar2=None, op0=ALU.mult), [qkg], [qkg])

        def load_weight(es, dst, src, ranges, gain, tag):
            wmax = max(b - a for a, b in ranges)
            es = ExitStack()
            stg = RR([mk(es, f"wstg{tag}{i}", [128, wmax]) for i in range(2)])
            for c in range(NCH):
                off = 0
                for (a, b) in ranges:
                    n = b - a
                    s = stg.next()
                    tk.dma("sp", lambda e, s=s, c=c, a=a, b=b, n=n: e.dma_start(out=s[:, 0:n], in_=src[c * 128:(c + 1) * 128, a:b]), [], [s], s)
                    if gain is None:
                        tk.op("act", lambda e, s=s, c=c, off=off, n=n: e.activation(out=dst[:, c, off:off + n], in_=s[:, 0:n], func=AF.Copy), [s], [dst])
                    else:
                        tk.op("act", lambda e, s=s, c=c, off=off, n=n: e.activation(out=dst[:, c, off:off + n], in_=s[:, 0:n], func=AF.Copy, scale=gain[:, c:c + 1]), [s, gain], [dst])
                    off += n
            tk.barrier()
            es.close()

        def rmsnorm_rows(xb_ap, xb_reg, junk, ss, rt, out_ap, out_reg, gain_bc=None):
            tk.op("act", lambda e: e.activation(out=junk[:], in_=xb_ap, func=AF.Square, accum_out=ss[:]), [xb_reg], [junk, ss])
            tk.op("act", lambda e: e.activation(out=rt[:], in_=ss[:], func=AF.Ln, scale=1.0 / D, bias=EPS), [ss], [rt])
            tk.op("act", lambda e: e.activation(out=rt[:], in_=rt[:], func=AF.Exp, scale=-0.5), [rt], [rt])
            if gain_bc is None:
                tk.op("dve", lambda e: e.tensor_scalar(out=out_ap, in0=xb_ap, scalar1=rt[:, 0:1], scalar2=None, op0=ALU.mult), [xb_reg, rt], [out_reg])
            else:
                tk.op("dve", lambda e: e.scalar_tensor_tensor(out=out_ap, in0=xb_ap, scalar=rt[:, 0:1], in1=gain_bc[:], op0=ALU.mult, op1=ALU.mult), [xb_reg, rt, gain_bc], [out_reg])

        with ExitStack() as es:
            wkv = mk(es, "wkv", [128, NCH, 512], BF16)
            load_weight(es, wkv, w_kv, [(0, 512)], gmem, "kv")
            mx = mk(es, "mx", [128, 2, D])
            mn = mk(es, "mn", [128, 2, D], BF16)
            mnT = mk(es, "mnT", [128, NCH, 256], BF16)
            junk = mk(es, "mjunk", [128, D], BF16)
            ss = mk(es, "mss", [128, 1]); rt = mk(es, "mrt", [128, 1])
            tp = RR([mk(es, f"mtp{i}", [128, 512], BF16, psum=True) for i in range(2)])
            kvps = RR([mk(es, f"mkv{i}", [128, 512], F32, psum=True) for i in range(2)])
            kvs = mk(es, "kvs", [128, 2, 512])
            kn = mk(es, "kn", [128, 2, 256], BF16)
            ss4 = mk(es, "ss4", [128, 4]); j64 = mk(es, "j64", [128, 64])
            tk.dma("sp", lambda e: e.dma_start(out=mx[:], in_=mem.rearrange("(j p) d -> p j d", p=128)), [], [mx], mx)
            for j in range(2):
                rmsnorm_rows(mx[:, j, :], mx, junk, ss, rt, mn[:, j, :], mn)
            for c in range(NCH):
                p = tp.next()
                for j in range(2):
                    tk.op("pe", lambda e, p=p, j=j, c=c: e.transpose(out=p[:, j * 128:(j + 1) * 128], in_=mn[:, j, c * 128:(c + 1) * 128], identity=ident[:]), [mn, ident], [p])
                evac(mnT[:, c, :], p[:, 0:256], [p], [mnT])
            for j in range(2):
                ps = kvps.next()
                for c in range(NCH):
                    tk.op("pe", lambda e, ps=ps, j=j, c=c: e.matmul(ps[:], lhsT=mnT[:, c, j * 128:(j + 1) * 128], rhs=wkv[:, c, :], start=(c == 0), stop=(c == NCH - 1)), [mnT, wkv], [ps])
                evac(kvs[:, j, :], ps[:], [ps], [kvs])
                tk.op("dve", lambda e, j=j: e.tensor_copy(out=memV[:, j, :], in_=kvs[:, j, 256:512]), [kvs], [memV])
                for h in range(4):
                    tk.op("act", lambda e, j=j, h=h: e.activation(out=j64[:], in_=kvs[:, j, h * 64:(h + 1) * 64], func=AF.Square, accum_out=ss4[:, h:h + 1]), [kvs], [j64, ss4])
                tk.op("act", lambda e: e.activation(out=ss4[:], in_=ss4[:], func=AF.Sqrt, scale=1.0 / 64, bias=EPS), [ss4], [ss4])
                tk.op("dve", lambda e: e.reciprocal(out=ss4[:], in_=ss4[:]), [ss4], [ss4])
                for h in range(4):
                    tk.op("dve", lambda e, j=j, h=h: e.tensor_scalar(out=kn[:, j, h * 64:(h + 1) * 64], in0=kvs[:, j, h * 64:(h + 1) * 64], scalar1=ss4[:, h:h + 1], scalar2=None, op0=ALU.mult), [kvs, ss4], [kn])
            for pr in range(2):
                p = tp.next()
                for j in range(2):
                    tk.op("pe", lambda e, p=p, j=j, pr=pr: e.transpose(out=p[:, j * 128:(j + 1) * 128], in_=kn[:, j, pr * 128:(pr + 1) * 128], identity=ident[:]), [kn, ident], [p])
                tk.op("dve", lambda e, p=p, pr=pr: e.tensor_scalar(out=memKT[:, pr, :], in0=p[:, 0:256], scalar1=qkg[:, 0:1], scalar2=None, op0=ALU.mult), [p, qkg], [memKT])
            tk.barrier()

        ss_stack = ExitStack()
        sslot = mk(ss_stack, "sslot", [128, NSLOT, 4, 64], BF16)
        tk.op("pool", lambda e: e.memset(sslot[:], 0.0), [], [sslot])

        if 1 in phases:
          with ExitStack() as es:
            W1 = mk(es, "W1", [128, NCH, 1792], BF16)
            load_weight(es, W1, w_in, [(512, 1536), (2048, 2816)], gmix, "1")
            xb = RR([mk(es, f"xb{i}", [128, 4, D]) for i in range(2)])
            xn = RR([mk(es, f"xn{i}", [128, 4, D], BF16) for i in range(2)])
            hT = RR([mk(es, f"hT{i}", [128, NCH, 512], BF16) for i in range(2)])
            junk = mk(es, "junk1", [128, D], BF16)
            ssr = RR([mk(es, f"ss1_{i}", [128, 1]) for i in range(4)])
            rtr = RR([mk(es, f"rt1_{i}", [128, 1]) for i in range(4)])
            tp = RR([mk(es, f"tp1_{i}", [128, 512], BF16, psum=True) for i in range(2)])
            mm = RR([mk(es, f"mm1_{i}", [128, 512], F32, psum=True) for i in range(4)])
            dsp = RR([mk(es, f"ds1_{i}", [128, 512], F32, psum=True) for i in range(2)])
            ktsb = RR([mk(es, f"ktsb{i}", [128, 512], BF16) for i in range(4)])
            vsb = RR([mk(es, f"vsb{i}", [128, 4, 512], BF16) for i in range(2)])
            vhgA = RR([mk(es, f"vhgA{i}", [128, 4, 256], BF16) for i in range(2)])
            vhgB = RR([mk(es, f"vhgB{i}", [128, 4, 256], BF16) for i in range(2)])
            for t_ in vhgA.tiles + vhgB.tiles:
                tk.op("pool", lambda e, t_=t_: e.memset(t_[:], 0.0), [], [t_])
            sg = RR([mk(es, f"sg{i}", [128, 512]) for i in range(2)])
            fb = RR([mk(es, f"fb{i}", [128, 512]) for i in range(2)])
            gb = RR([mk(es, f"gb{i}", [128, 512]) for i in range(2)])
            bc = RR([mk(es, f"bc{i}", [128, 512]) for i in range(2)])
            em = RR([mk(es, f"em{i}", [128, 512]) for i in range(2)])
            ktl = RR([mk(es, f"ktl{i}", [128, 512], BF16) for i in range(8)])
            ktT = RR([mk(es, f"ktT{i}", [128, 4, 128], BF16) for i in range(8)])
            el = RR([mk(es, f"el{i}", [128, 8]) for i in range(8)])
            S = [mk(es, f"S{h}", [128, 64]) for h in range(4)]
            Stmp = RR([mk(es, f"Stmp{i}", [128, 64]) for i in range(2)])
            for h in range(4):
                tk.op("dve", lambda e, h=h: e.memset(S[h][:], 0.0), [], [S[h]])

            def load_x(T):
                t = xb.next()
                tk.dma("sp", lambda e: e.dma_start(out=t[:], in_=xk[T * 512:(T + 1) * 512, :].rearrange("(j p) d -> p j d", p=128)), [], [t], t)
                return t
            pend = [None]

            def run_b2(T, hp, vh):
                hp2 = []
                for hd in range(4):
                    k_, kT_, el_ = hp[hd]
                    p = tp.next()
                    for j in range(4):
                        tk.op("pe", lambda e, j=j: e.transpose(out=p[:, j * 128:(j + 1) * 128], in_=k_[:, j * 128:(j + 1) * 128], identity=ident[:]), [k_, ident], [p])
                    evac(kT_[:], p[:].rearrange("p (j k) -> p j k", j=4), [p], [kT_])
                    hp2.append((kT_, el_))
                hp = hp2
                for h0 in (0, 2):
                    banks = {}
                    for hd in (h0, h0 + 1):
                        kT_, el_ = hp[hd]
                        dps = dsp.next(); banks[hd] = dps
                        for ch in range(8):
                            j, half = ch // 2, ch % 2
                            tk.op("pe", lambda e, ch=ch, j=j, half=half: e.matmul(dps[:, ch * 64:(ch + 1) * 64], lhsT=kT_[:, j, :], rhs=vh[half][:, j, hd * 64:(hd + 1) * 64], start=True, stop=True), [kT_, vh[half]], [dps])
                    for ch in range(8):
                        j, half = ch // 2, ch % 2
                        for hd in (h0, h0 + 1):
                            kT_, el_ = hp[hd]; dps = banks[hd]
                            tk.op("dve", lambda e, ch=ch: e.scalar_tensor_tensor(out=S[hd][:], in0=S[hd][:], scalar=el_[:, ch:ch + 1], in1=dps[:, ch * 64:(ch + 1) * 64], op0=ALU.mult, op1=ALU.add), [S[hd], el_, dps], [S[hd]])
                            if half == 1:
                                B = T * 4 + j + 1
                                if B < NBLK:
                                    m_, r_ = B // 4, B % 4
                                    sl = 2 * m_ + (0 if r_ < 2 else 1)
                                    tk.op("dve", lambda e, sl=sl, r_=r_: e.scalar_tensor_tensor(out=sslot[:, sl, hd, :], in0=S[hd][:], scalar=blend[:, r_:r_ + 1], in1=sslot[:, sl, hd, :], op0=ALU.mult, op1=ALU.add), [S[hd], blend, sslot], [sslot])


            nxt = load_x(0)
            for T in range(NT1):
                xt = nxt
                if T + 1 < NT1:
                    nxt = load_x(T + 1)
                xnt = xn.next(); h = hT.next()
                for j in range(4):
                    rmsnorm_rows(xt[:, j, :], xt, junk, ssr.next(), rtr.next(), xnt[:, j, :], xnt)
                for c in range(NCH):
                    p = tp.next()
                    for j in range(4):
                        tk.op("pe", lambda e, p=p, j=j, c=c: e.transpose(out=p[:, j * 128:(j + 1) * 128], in_=xnt[:, j, c * 128:(c + 1) * 128], identity=ident[:]), [xnt, ident], [p])
                    evac(h[:, c, :], p[:], [p], [h])
                for pr in range(4):
                    ps = mm.next()
                    for c in range(NCH):
                        tk.op("pe", lambda e, ps=ps, c=c, pr=pr: e.matmul(ps[:], lhsT=W1[:, c, pr * 128:(pr + 1) * 128], rhs=h[:, c, :], start=(c == 0), stop=(c == NCH - 1)), [W1, h], [ps])
                    ks = ktsb.next()
                    evac(ks[:], ps[:], [ps], [ks])
                    tk.dma("sp", lambda e, ks=ks, pr=pr: e.dma_start(out=KT_d[pr, :, T * 512:(T + 1) * 512], in_=ks[:]), [ks], [R_KT], ks)
                vs = vsb.next()
                for j in range(4):
                    ps = mm.next()
                    for c in range(NCH):
                        tk.op("pe", lambda e, ps=ps, c=c, j=j: e.matmul(ps[:], lhsT=h[:, c, j * 128:(j + 1) * 128], rhs=W1[:, c, 512:1024], start=(c == 0), stop=(c == NCH - 1)), [W1, h], [ps])
                    evac(vs[:, j, :], ps[:], [ps], [vs])
                tk.dma("sp", lambda e, vs=vs: e.dma_start(out=V_d[T * 4:(T + 1) * 4].rearrange("j p c -> p j c"), in_=vs[:]), [vs], [R_V], vs)
                vh = (vhgA.next(), vhgB.next())
                for j in range(4):
                    ps = mm.next()
                    for c in range(NCH):
                        tk.op("pe", lambda e, ps=ps, c=c, j=j: e.matmul(ps[:, 0:256], lhsT=h[:, c, j * 128:(j + 1) * 128], rhs=W1[:, c, 1536:1792], start=(c == 0), stop=(c == NCH - 1)), [W1, h], [ps])
                    evac(vh[0][0:64, j, :], ps[0:64, 0:256], [ps], [vh[0]])
                    evac(vh[1][64:128, j, :], ps[64:128, 0:256], [ps], [vh[1]])
                hp = []
                for hd in range(4):
                    ps = mm.next()
                    for c in range(NCH):
                        tk.op("pe", lambda e, ps=ps, c=c, hd=hd: e.matmul(ps[:], lhsT=W1[:, c, 1024 + hd * 128:1024 + (hd + 1) * 128], rhs=h[:, c, :], start=(c == 0), stop=(c == NCH - 1)), [W1, h], [ps])
                    s_ = sg.next(); f_ = fb.next(); g_ = gb.next(); b_ = bc.next(); e_ = em.next(); k_ = ktl.next(); kT_ = ktT.next(); el_ = el.next()
                    tk.op("act", lambda e: e.activation(out=s_[:], in_=ps[:], func=AF.Sigmoid), [ps], [s_])
                    tk.op("dve", lambda e: e.tensor_scalar(out=f_[:], in0=s_[:], scalar1=oml[:, hd:hd + 1], scalar2=lb[:, hd:hd + 1], op0=ALU.mult, op1=ALU.add), [s_, oml, lb], [f_])
                    tk.op("act", lambda e: e.activation(out=g_[:], in_=f_[:], func=AF.Ln), [f_], [g_])
                    tk.op("dve", lambda e: e.tensor_tensor_scan(out=b_[:], data0=resetm[:], data1=g_[:], initial=0.0, op0=ALU.mult, op1=ALU.add), [resetm, g_], [b_])
                    for ch in range(8):
                        tk.op("act", lambda e, ch=ch: e.activation(out=e_[:, ch * 64:(ch + 1) * 64], in_=b_[:, ch * 64:(ch + 1) * 64], func=AF.Exp, scale=-1.0, bias=b_[:, ch * 64 + 63:ch * 64 + 64]), [b_], [e_])
                    tk.op("act", lambda e: e.activation(out=el_[:], in_=b_[:, 63::64], func=AF.Exp), [b_], [el_])
                    tk.op("dve", lambda e: e.tensor_tensor(out=f_[:], in0=f_[:], in1=e_[:], op=ALU.mult), [f_, e_], [f_])
                    tk.op("dve", lambda e: e.tensor_tensor(out=k_[:], in0=e_[:], in1=f_[:], op=ALU.subtract), [f_, e_], [k_])
                    hp.append((k_, kT_, el_))
                pend_now = (T, hp, vh)
                if pend[0] is not None:
                    run_b2(*pend[0])
                pend[0] = pend_now
            run_b2(*pend[0])
            tk.barrier()

        if 2 in phases:
          with ExitStack() as es:
            W2 = mk(es, "W2", [128, NCH, 2304], BF16)
            load_weight(es, W2, w_in, [(0, 512), (1536, 2560), (2560, 3328)], gmix, "2")
            xb = RR([mk(es, f"xb2_{i}", [128, D]) for i in range(2)])
            xn = RR([mk(es, f"xn2_{i}", [128, D], BF16) for i in range(2)])
            hT = RR([mk(es, f"hT2_{i}", [128, NCH, 128], BF16) for i in range(2)])
            junk = mk(es, "junk2", [128, D], BF16)
            ssr = RR([mk(es, f"ss2_{i}", [128, 1]) for i in range(2)])
            rtr = RR([mk(es, f"rt2_{i}", [128, 1]) for i in range(2)])
            tp = RR([mk(es, f"tp2_{i}", [128, 512], BF16, psum=True) for i in range(2)])
            mm = RR([mk(es, f"mm2_{i}", [128, 512], F32, psum=True) for i in range(5)])
            op_ = mk(es, "op2", [128, 512], F32, psum=True)
            qts = RR([mk(es, f"qts{i}", [128, 4, 128], BF16) for i in range(2)])
            mix = RR([mk(es, f"mix2_{i}", [128, 512], BF16) for i in range(2)])
            tok = RR([mk(es, f"tok2_{i}", [128, 768]) for i in range(2)])
            vown = RR([mk(es, f"vown{i}", [128, 256], BF16) for i in range(2)])
            mqT = RR([mk(es, f"mqT{i}", [128, 2, 128], BF16) for i in range(2)])
            t512 = RR([mk(es, f"t512_{i}", [128, 512]) for i in range(8)])
            b512 = RR([mk(es, f"b512_{i}", [128, 512], BF16) for i in range(6)])
            sc_sb = RR([mk(es, f"scsb{i}", [128, 128], BF16) for i in range(3)])
            kcAr = RR([mk(es, f"kcA{i}", [128, 4, 128], BF16) for i in range(2)])
            kcBr = RR([mk(es, f"kcB{i}", [128, 4, 128], BF16) for i in range(2)])
            for t_ in kcAr.tiles + kcBr.tiles:
                tk.op("pool", lambda e, t_=t_: e.memset(t_[:], 0.0), [], [t_])
            pbf = RR([mk(es, f"pbf{i}", [128, 256], BF16) for i in range(2)])
            pT = RR([mk(es, f"pT{i}", [128, 2, 128], BF16) for i in range(2)])
            small = RR([mk(es, f"sm2_{i}", [128, 8]) for i in range(8)])
            j64 = mk(es, "j64b", [128, 64])
            o_sb = RR([mk(es, f"osb{i}", [128, 256]) for i in range(2)])
            sgate = RR([mk(es, f"sgate{i}", [128, 256]) for i in range(2)])

            def load_x(j):
                t = xb.next()
                tk.dma("sp", lambda e: e.dma_start(out=t[:], in_=xq[j * 128:(j + 1) * 128, :]), [], [t], t)
                return t
            nxt = load_x(0)
            for sl in range(NSLOT):
                xt = nxt
                if sl + 1 < NSLOT:
                    nxt = load_x(sl + 1)
                xnt = xn.next(); h = hT.next()
                rmsnorm_rows(xt[:], xt, junk, ssr.next(), rtr.next(), xnt[:], xnt)
                for half in range(2):
                    p = tp.next()
                    for c4 in range(4):
                        c = half * 4 + c4
                        tk.op("pe", lambda e, p=p, c=c, c4=c4: e.transpose(out=p[:, c4 * 128:(c4 + 1) * 128], in_=xnt[:, c * 128:(c + 1) * 128], identity=ident[:]), [xnt, ident], [p])
                    evac(h[:, half * 4:(half + 1) * 4, :], p[:].rearrange("p (c t) -> p c t", c=4), [p], [h])

                def fm_proj(col0, ngrp):
                    ps = mm.next()
                    for g in range(ngrp):
                        for c in range(NCH):
                            tk.op("pe", lambda e, g=g, c=c: e.matmul(ps[:, g * 128:(g + 1) * 128], lhsT=W2[:, c, col0 + g * 128:col0 + (g + 1) * 128], rhs=h[:, c, :], start=(c == 0), stop=(c == NCH - 1)), [W2, h], [ps])
                    return ps
                ps = fm_proj(0, 4)
                qt = qts.next()
                evac(qt[:], ps[:].rearrange("p (g t) -> p g t", g=4), [ps], [qt])
                tk.dma("sp", lambda e, qt=qt: e.dma_start(out=QT_d[:, :, sl * 128:(sl + 1) * 128].rearrange("g p t -> p g t"), in_=qt[:]), [qt], [R_QT], qt)
                tkt = tok.next()
                psA = mm.next(); psB = mm.next()
                for c in range(NCH):
                    tk.op("pe", lambda e, c=c: e.matmul(psA[:], lhsT=h[:, c, :], rhs=W2[:, c, 1536:2048], start=(c == 0), stop=(c == NCH - 1)), [W2, h], [psA])
                for c in range(NCH):
                    tk.op("pe", lambda e, c=c: e.matmul(psB[:, 0:256], lhsT=h[:, c, :], rhs=W2[:, c, 2048:2304], start=(c == 0), stop=(c == NCH - 1)), [W2, h], [psB])
                evac(tkt[:, 0:512], psA[:], [psA], [tkt])
                evac(tkt[:, 512:768], psB[:, 0:256], [psB], [tkt])
                vo = vown.next()
                tk.op("dve", lambda e: e.tensor_copy(out=vo[:], in_=tkt[:, 0:256]), [tkt], [vo])
                psq = fm_proj(512, 4)
                psf = fm_proj(1024, 4)
                sig = t512.next(); f_ = t512.next(); g_ = t512.next(); b_ = t512.next()
                tk.op("act", lambda e: e.activation(out=sig[:], in_=psf[:], func=AF.Sigmoid), [psf], [sig])
                sq = t512.next()
                tk.op("act", lambda e: e.activation(out=sq[:], in_=psq[:], func=AF.Silu), [psq], [sq])
                sgt = sgate.next()
                tk.op("act", lambda e: e.activation(out=sgt[:], in_=tkt[:, 256:512], func=AF.Silu), [tkt], [sgt])
                for hd in range(4):
                    tk.op("dve", lambda e, hd=hd: e.tensor_scalar(out=f_[:, hd * 128:(hd + 1) * 128], in0=sig[:, hd * 128:(hd + 1) * 128], scalar1=oml[:, hd:hd + 1], scalar2=lb[:, hd:hd + 1], op0=ALU.mult, op1=ALU.add), [sig, oml, lb], [f_])
                tk.op("act", lambda e: e.activation(out=g_[:], in_=f_[:], func=AF.Ln), [f_], [g_])
                for hd in range(4):
                    tk.op("dve", lambda e, hd=hd: e.tensor_tensor_scan(out=b_[:, hd * 128:(hd + 1) * 128], data0=ones[:, 0:128], data1=g_[:, hd * 128:(hd + 1) * 128], initial=0.0, op0=ALU.mult, op1=ALU.add), [ones, g_], [b_])
                mmid = small.next(); nmid = small.next()
                tk.op("dve", lambda e: e.tensor_copy(out=mmid[:, 0:4], in_=b_[:, 63::128]), [b_], [mmid])
                tk.op("dve", lambda e: e.tensor_scalar(out=nmid[:, 0:4], in0=mmid[:, 0:4], scalar1=-1.0, scalar2=None, op0=ALU.mult), [mmid], [nmid])
                ecp = t512.next(); ecm = t512.next(); ep = t512.next()
                for hd in range(4):
                    sl_ = slice(hd * 128, (hd + 1) * 128)
                    tk.op("act", lambda e, hd=hd, sl_=sl_: e.activation(out=ecp[:, sl_], in_=b_[:, sl_], func=AF.Exp, bias=nmid[:, hd:hd + 1]), [b_, nmid], [ecp])
                    tk.op("act", lambda e, hd=hd, sl_=sl_: e.activation(out=ecm[:, sl_], in_=b_[:, sl_], func=AF.Exp, scale=-1.0, bias=mmid[:, hd:hd + 1]), [b_, mmid], [ecm])
                tk.op("act", lambda e: e.activation(out=ep[:], in_=b_[:], func=AF.Exp), [b_], [ep])
                qc = b512.next(); qh = b512.next()
                tk.op("dve", lambda e: e.tensor_tensor(out=qc[:], in0=sq[:], in1=ecp[:], op=ALU.mult), [sq, ecp], [qc])
                tk.op("dve", lambda e: e.tensor_tensor(out=qh[:], in0=sq[:], in1=ep[:], op=ALU.mult), [sq, ep], [qh])
                tk.op("dve", lambda e: e.tensor_tensor(out=f_[:], in0=f_[:], in1=ecm[:], op=ALU.mult), [f_, ecm], [f_])
                kcA = kcAr.next(); kcB = kcBr.next()
                ecv = ecm[:].rearrange("p (h s) -> p h s", h=4); fv = f_[:].rearrange("p (h s) -> p h s", h=4)
                tk.op("dve", lambda e: e.tensor_tensor(out=kcA[:, :, 0:64], in0=ecv[:, :, 0:64], in1=fv[:, :, 0:64], op=ALU.subtract), [f_, ecm], [kcA])
                tk.op("dve", lambda e: e.tensor_tensor(out=kcB[:, :, 64:128], in0=ecv[:, :, 64:128], in1=fv[:, :, 64:128], op=ALU.subtract), [f_, ecm], [kcB])
                for hd in range(4):
                    sl_ = slice(hd * 128, (hd + 1) * 128)
                    sps = mm.next()
                    tk.op("pe", lambda e, sl_=sl_, sps=sps, hd=hd: e.matmul(sps[:, 0:128], lhsT=kcA[:, hd, :], rhs=qc[:, sl_], start=True, stop=False), [kcA, qc], [sps])
                    tk.op("pe", lambda e, sps=sps, hd=hd: e.matmul(sps[:, 64:128], lhsT=kcB[:, hd, :], rhs=qc[:, hd * 128 + 64:hd * 128 + 128], start=False, stop=True), [kcB, qc], [sps])
                    scs = sc_sb.next()
                    tk.op("dve", lambda e, sps=sps, scs=scs: e.tensor_tensor(out=scs[:], in0=sps[:, 0:128], in1=maskst[:], op=ALU.mult), [sps, maskst], [scs])
                    tk.op("pe", lambda e, scs=scs, hd=hd: e.matmul(op_[:, hd * 64:(hd + 1) * 64], lhsT=scs[:], rhs=vo[:, hd * 64:(hd + 1) * 64], start=True, stop=False), [scs, vo], [op_])
                    tk.op("pe", lambda e, sl_=sl_, hd=hd: e.matmul(op_[:, hd * 64:(hd + 1) * 64], lhsT=qh[:, sl_], rhs=sslot[:, sl, hd, :], start=False, stop=True), [qh, sslot], [op_])
                osb = o_sb.next()
                evac(osb[:], op_[:, 0:256], [op_], [osb])
                ss4 = small.next()
                for hd in range(4):
                    tk.op("act", lambda e, hd=hd: e.activation(out=j64[:], in_=osb[:, hd * 64:(hd + 1) * 64], func=AF.Square, accum_out=ss4[:, hd:hd + 1]), [osb], [j64, ss4])
                tk.op("act", lambda e: e.activation(out=ss4[:, 0:4], in_=ss4[:, 0:4], func=AF.Ln, scale=1.0 / 64, bias=EPS), [ss4], [ss4])
                tk.op("act", lambda e: e.activation(out=ss4[:, 0:4], in_=ss4[:, 0:4], func=AF.Exp, scale=-0.5), [ss4], [ss4])
                mx_ = mix.next()
                for hd in range(4):
                    tk.op("dve", lambda e, hd=hd: e.scalar_tensor_tensor(out=mx_[:, hd * 64:(hd + 1) * 64], in0=osb[:, hd * 64:(hd + 1) * 64], scalar=ss4[:, hd:hd + 1], in1=sgt[:, hd * 64:(hd + 1) * 64], op0=ALU.mult, op1=ALU.mult), [osb, ss4, sgt], [mx_])
                psm = fm_proj(2048, 2)
                mq = mqT.next()
                evac(mq[:], psm[:, 0:256].rearrange("p (g t) -> p g t", g=2), [psm], [mq])
                ssq = small.next()
                for hd in range(4):
                    tk.op("act", lambda e, hd=hd: e.activation(out=j64[:], in_=tkt[:, 512 + hd * 64:512 + (hd + 1) * 64], func=AF.Square, accum_out=ssq[:, hd:hd + 1]), [tkt], [j64, ssq])
                tk.op("act", lambda e: e.activation(out=ssq[:, 0:4], in_=ssq[:, 0:4], func=AF.Ln, scale=1.0 / 64, bias=EPS), [ssq], [ssq])
                tk.op("act", lambda e: e.activation(out=ssq[:, 0:4], in_=ssq[:, 0:4], func=AF.Exp, scale=-0.5), [ssq], [ssq])
                rs = small.next()
                omp = mm.next()
                for hd in range(4):
                    pr, ph = hd // 2, (hd % 2) * 64
                    lps = mm.next()
                    tk.op("pe", lambda e, lps=lps, pr=pr, ph=ph: e.matmul(lps[:, 0:256], lhsT=mq[ph:ph + 64, pr, :], rhs=memKT[ph:ph + 64, pr, :], start=True, stop=True), [mq, memKT], [lps])
                    pb = pbf.next()
                    tk.op("act", lambda e, lps=lps, pb=pb, hd=hd: e.activation(out=pb[:], in_=lps[:, 0:256], func=AF.Exp, scale=ssq[:, hd:hd + 1], accum_out=rs[:, hd:hd + 1]), [lps, ssq], [pb, rs])
                    p = tp.next()
                    for mb in range(2):
                        tk.op("pe", lambda e, p=p, pb=pb, mb=mb: e.transpose(out=p[:, mb * 128:(mb + 1) * 128], in_=pb[:, mb * 128:(mb + 1) * 128], identity=ident[:]), [pb, ident], [p])
                    pt_ = pT.next()
                    evac(pt_[:], p[:, 0:256].rearrange("p (m t) -> p m t", m=2), [p], [pt_])
                    for mb in range(2):
                        tk.op("pe", lambda e, pt_=pt_, mb=mb, hd=hd: e.matmul(omp[:, hd * 64:(hd + 1) * 64], lhsT=pt_[:, mb, :], rhs=memV[:, mb, hd * 64:(hd + 1) * 64], start=(mb == 0), stop=(mb == 1)), [pt_, memV], [omp])
                tk.op("dve", lambda e: e.reciprocal(out=rs[:, 0:4], in_=rs[:, 0:4]), [rs], [rs])
                for hd in range(4):
                    tk.op("dve", lambda e, hd=hd: e.tensor_scalar(out=mx_[:, 256 + hd * 64:256 + (hd + 1) * 64], in0=omp[:, hd * 64:(hd + 1) * 64], scalar1=rs[:, hd:hd + 1], scalar2=None, op0=ALU.mult), [omp, rs], [mx_])
                tk.dma("sp", lambda e, mx_=mx_: e.dma_start(out=MIX_d[sl * 128:(sl + 1) * 128, 512:1024], in_=mx_[:]), [mx_], [R_MIX], mx_)
            tk.barrier()

        tk.barrier()
        ss_stack.close()

        if 3 in phases:
          with ExitStack() as es:
            KT = RR([mk(es, f"KT{i}", [128, SEQ], BF16) for i in range(2)])
            Vp = RR([mk(es, f"Vp{i}", [128, NBLK, 128], BF16) for i in range(2)])
            QA = RR([mk(es, f"QA{i}", [128, NOWN], BF16) for i in range(2)])
            QB = RR([mk(es, f"QB{i}", [128, NOWN], BF16) for i in range(2)])
            for t_ in QA.tiles + QB.tiles:
                tk.op("pool", lambda e, t_=t_: e.memset(t_[:], 0.0), [], [t_])
            Pu = RR([mk(es, f"Pu{i}", [128, 513]) for i in range(8)])
            zps = RR([mk(es, f"zps{i}", [128, 512], F32, psum=True) for i in range(3)])
            atp = RR([mk(es, f"atp{i}", [128, 512], BF16, psum=True) for i in range(2)])
            ops = RR([mk(es, f"ops{i}", [128, 64], F32, psum=True) for i in range(3)])
            gbuf = RR([mk(es, f"gbuf{i}", [128, 512]) for i in range(6)])
            abuf = RR([mk(es, f"abuf{i}", [128, 512], BF16) for i in range(6)])
            aT = RR([mk(es, f"aT{i}", [128, 512], BF16) for i in range(6)])
            sbo = RR([mk(es, f"sbo{i}", [128, 128], BF16) for i in range(4)])
            ssr = RR([mk(es, f"ss3_{i}", [128, 1]) for i in range(4)])
            j64 = mk(es, "j64c", [128, 64])
            oraw = mk(es, "oraw", [128, NSLOT, 128])
            obf = mk(es, "obf", [128, NSLOT, 128], BF16)
            ssq = mk(es, "ssq3", [128, 2 * NSLOT])
            oraw_regs = [Reg(f"oraw_{i}") for i in range(2 * NSLOT)]
            dcnt = [0]
            negmask = mk(es, "negmask", [128, 2, 256], BF16)
            tk.op("dve", lambda e: e.tensor_scalar(out=negmask[:], in0=maskg[:], scalar1=-30000.0, scalar2=None, op0=ALU.mult), [maskg], [negmask])
            for pr in range(4):
                kt = KT.next(); vp = Vp.next(); qa = QA.next(); qb = QB.next()
                tk.dma("sp", lambda e: e.dma_start(out=kt[:], in_=KT_d[pr]), [R_KT], [kt], kt)
                for jq in range(0, NBLK, 16):
                    j1 = min(NBLK, jq + 16)
                    tk.dma("sp", lambda e: e.dma_start(out=vp[:, jq:j1, :], in_=V_d[jq:j1, :, pr * 128:(pr + 1) * 128].rearrange("j p c -> p j c")), [R_V], [vp], vp)
                tk.dma("sp", lambda e: e.dma_start(out=qa[0:64, :], in_=QT_d[pr, 0:64, :]), [R_QT], [qa], qa)
                tk.dma("sp", lambda e: e.dma_start(out=qb[64:128, :], in_=QT_d[pr, 64:128, :]), [R_QT], [qb], qb)
                qpad = (qa, qb)
                U = []
                for sl in range(NSLOT):
                    m_, par = sl // 2, sl % 2
                    nb = 4 * m_ + (2 if par == 0 else 4)
                    nk = nb * 128
                    units = [(nk - 256, nk, True)]
                    hi = nk - 256
                    while hi > 0:
                        lo = max(0, hi - 512)
                        units.append((lo, hi, False))
                        hi = lo
                    grps = [{"sl": sl, "hh": hh, "nk": nk, "par": par, "nb": nb, "prev": None} for hh in range(2)]
                    bd = 0
                    for ui, (k0, k1, masked) in enumerate(units):
                        for hh in range(2):
                            U.append({"g": grps[hh], "k0": k0, "k1": k1, "m": masked, "first": ui == 0,
                                      "last": ui == len(units) - 1, "bd": bd})
                        bd += (k1 - k0) // 128
                so_by_slot = {}

                def stA(u):
                    g_ = u["g"]; ph = g_["hh"] * 64; sl = g_["sl"]; w = u["k1"] - u["k0"]
                    if u["first"]:
                        g_["o"] = ops.next()
                    z = zps.next()
                    qp = qpad[g_["hh"]]
                    tk.op("pe", lambda e: e.matmul(z[:, 0:w], lhsT=qp[:, sl * 128:(sl + 1) * 128], rhs=kt[:, u["k0"]:u["k1"]], start=True, stop=not u["m"]), [qp, kt], [z])
                    if u["m"]:
                        tk.op("pe", lambda e: e.matmul(z[:, 0:256], lhsT=ident[:], rhs=negmask[:, g_["par"], :], start=False, stop=True), [ident, negmask], [z])
                    g = gbuf.next()
                    tk.op("act", lambda e: e.activation(out=g[:, 0:w], in_=z[:, 0:w], func=AF.Sigmoid, scale=-0.125), [z], [g])
                    u["gb"] = g

                def stB(u):
                    g_ = u["g"]; k0, k1 = u["k0"], u["k1"]; w = k1 - k0; g = u["gb"]
                    P_ = Pu.next(); prev = g_["prev"]
                    if not hasattr(P_, "rc"):
                        P_.rc = Reg(P_.r.name + "_c")
                    if prev is None:
                        tk.op("pool", lambda e: e.memset(P_[:, w:w + 1], 1.0), [], [P_.rc])
                        tk.op("dve", lambda e: e.tensor_tensor_scan(out=P_[:, 0:w][:, ::-1], data0=g[:, 0:w][:, ::-1], data1=ones[:, 0:w], initial=1.0, op0=ALU.mult, op1=ALU.mult), [g, ones], [P_])
                    else:
                        tk.op("act", lambda e: e.activation(out=P_[:, w:w + 1], in_=prev[:, 0:1], func=AF.Copy), [prev], [P_.rc])
                        tk.op("dve", lambda e: e.tensor_tensor_scan(out=P_[:, 0:w][:, ::-1], data0=g[:, 0:w][:, ::-1], data1=ones[:, 0:w], initial=prev[:, 0:1], op0=ALU.mult, op1=ALU.mult), [g, ones, prev], [P_])
                    g_["prev"] = P_
                    a = abuf.next()
                    dcnt[0] += 1
                    de = "pool"
                    tk.op(de, lambda e: e.tensor_tensor(out=a[:, 0:w], in0=P_[:, 1:w + 1], in1=P_[:, 0:w], op=ALU.subtract), [P_, P_.rc], [a])
                    u["a"] = a

                def stC(u):
                    w = u["k1"] - u["k0"]; a = u["a"]
                    tp_ = atp.next()
                    for b in range(w // 128):
                        tk.op("pe", lambda e, b=b: e.transpose(out=tp_[:, b * 128:(b + 1) * 128], in_=a[:, b * 128:(b + 1) * 128], identity=ident[:]), [a, ident], [tp_])
                    at = aT.next()
                    tk.op("act", lambda e: e.activation(out=at[:, 0:w], in_=tp_[:, 0:w], func=AF.Copy), [tp_], [at])
                    u["at"] = at

                def stD(u):
                    g_ = u["g"]; ph = g_["hh"] * 64; sl = g_["sl"]; w = u["k1"] - u["k0"]; at = u["at"]; o_ps = g_["o"]
                    for b in range(w // 128):
                        kb = u["k0"] // 128 + b
                        bd = u["bd"] + b
                        tk.op("pe", lambda e, b=b, kb=kb, bd=bd: e.matmul(o_ps[:], lhsT=at[:, b * 128:(b + 1) * 128], rhs=vp[:, kb, ph:ph + 64], start=(bd == 0), stop=(bd == g_["nb"] - 1)), [at, vp], [o_ps])
                    if u["last"]:
                        hh = g_["hh"]
                        rg = oraw_regs[sl * 2 + hh]
                        tk.op("dve", lambda e: e.tensor_copy(out=oraw[:, sl, ph:ph + 64], in_=o_ps[:]), [o_ps], [rg])
                        tk.op("dve", lambda e: e.scalar_tensor_tensor(out=j64[:], in0=oraw[:, sl, ph:ph + 64], scalar=1.0, in1=oraw[:, sl, ph:ph + 64], op0=ALU.mult, op1=ALU.mult, accum_out=ssq[:, sl * 2 + hh:sl * 2 + hh + 1]), [rg], [j64, rg])

                N = len(U)
                SK = 2
                for i in range(N + 3 * SK):
                    if i < N:
                        stA(U[i])
                    if 0 <= i - SK < N:
                        stB(U[i - SK])
                    if 0 <= i - 2 * SK < N:
                        stC(U[i - 2 * SK])
                    if 0 <= i - 3 * SK < N:
                        stD(U[i - 3 * SK])
                tk.op("act", lambda e: e.activation(out=ssq[:], in_=ssq[:], func=AF.Sqrt, scale=1.0 / 64, bias=EPS), oraw_regs, [ssq])
                tk.op("dve", lambda e: e.reciprocal(out=ssq[:], in_=ssq[:]), [ssq], [ssq])
                tk.op("dve", lambda e: e.tensor_tensor(out=obf[:].rearrange("p s (h d) -> p s h d", h=2), in0=oraw[:].rearrange("p s (h d) -> p s h d", h=2), in1=ssq[:].rearrange("p (s h) -> p s h", h=2).unsqueeze(3).broadcast_to([128, NSLOT, 2, 64]), op=ALU.mult), oraw_regs + [ssq], [obf])
                tk.dma("sp", lambda e: e.dma_start(out=MIX_d[:, pr * 128:(pr + 1) * 128].rearrange("(s p) c -> p s c", p=128), in_=obf[:]), [obf], [R_MIX], obf)
            tk.barrier()

        if 4 in phases:
          with ExitStack() as es:
            NG = 11
            Wo = mk(es, "Wo", [128, NCH, D], BF16)
            load_weight(es, Wo, w_out, [(0, 512), (512, 1024)], gout, "o")
            Wq = mk(es, "Wq", [128, NCH, 2048], BF16)
            load_weight(es, Wq, w_pq, [(0, 1024), (1024, 2048)], None, "q")
            kT = mk(es, "kT", [128, 16, 128], BF16)
            with ExitStack() as es2:
                kstg = mk(es2, "kstg", [128, 16, 128])
                tk.dma("sp", lambda e: e.dma_start(out=kstg[:], in_=keysT), [], [kstg], kstg)
                tk.op("dve", lambda e: e.tensor_copy(out=kT[:], in_=kstg[:]), [kstg], [kT])
                tk.barrier()
            gffn = mk(es, "gffn", [128, D])
            tk.dma("sp", lambda e: e.dma_start(out=gffn[:], in_=g_ffn_bc), [], [gffn], gffn)
            xb = RR([mk(es, f"xb4_{i}", [128, D]) for i in range(2)])
            mxb = RR([mk(es, f"mxb{i}", [128, D], BF16) for i in range(2)])
            mT = mk(es, "mT", [128, NCH, 128], BF16)
            x1 = RR([mk(es, f"x1_{i}", [128, D]) for i in range(2)])
            h2 = RR([mk(es, f"h2_{i}", [128, D], BF16) for i in range(2)])
            h2T = mk(es, "h2T", [128, NCH, 128], BF16)
            junk = mk(es, "junk4", [128, D], BF16)
            ssr = RR([mk(es, f"ss4_{i}", [128, 1]) for i in range(2)])
            rtr = RR([mk(es, f"rt4_{i}", [128, 1]) for i in range(2)])
            yps = [mk(es, f"yps{i}", [128, 512], F32, psum=True) for i in range(2)]
            tps = mk(es, "tps4", [128, 1024], BF16, psum=True)
            wps = [mk(es, f"wps{i}", [128, 512], F32, psum=True) for i in range(2)]
            qsp = RR([mk(es, f"qsp{i}", [128, 512], F32, psum=True) for i in range(3)])
            qg = mk(es, "qg", [128, 16, 128], BF16)
            sc = mk(es, "sc", [128, 16, 128])
            sc2 = mk(es, "sc2", [128, 16, 128])
            tops = mk(es, "tops", [128, 16, 16])
            topi = mk(es, "topi", [128, 16, 16], U32)
            topf = mk(es, "topf", [128, 16, 16])
            cand = mk(es, "cand", [128, 8, 256])
            cand2 = mk(es, "cand2", [128, 8, 256])
            best = mk(es, "best", [128, 8, 16])
            pos = mk(es, "pos", [128, 8, 16], U32)
            posa = mk(es, "posa", [128, 8, 16], U32)
            posb = mk(es, "posb", [128, 8, 16], U32)
            paf = mk(es, "paf", [128, 8, 16])
            pbf_ = mk(es, "pbf_", [128, 8, 16])
            eq = mk(es, "eq", [128, 8, 16, 16])
            i1s = mk(es, "i1s", [128, 8, 16])
            i2s = mk(es, "i2s", [128, 8, 16])
            eidf = mk(es, "eidf", [128, 128])
            eidxr = RR([mk(es, f"eidx{i}", [128, 128], U32) for i in range(2)])
            gater = RR([mk(es, f"gate{i}", [128, 8, 16]) for i in range(2)])
            gsum = mk(es, "gsum", [128, 8])
            actr = [mk(es, f"actr{i}", [128, 128]) for i in range(2)]
            glr = [mk(es, f"glr{i}", [128, 128]) for i in range(2)]
            actr_regs = [[Reg(f"actr{i}_{s}") for s in range(128)] for i in range(2)]
            glr_regs = [[Reg(f"glr{i}_{s}") for s in range(128)] for i in range(2)]
            wr = [mk(es, f"wr{i}", [128, 128]) for i in range(2)]
            wr_regs = [[Reg(f"wr{i}_{s}") for s in range(128)] for i in range(2)]
            uvb = RR([mk(es, f"uvb{i}", [128, 2 * D], BF16) for i in range(NG)])
            dg = RR([mk(es, f"dg{i}", [128, 128], BF16) for i in range(6)])
            ttj = mk(es, "ttj", [128, D], BF16)
            prodb = RR([mk(es, f"prodb{i}", [128, D], BF16) for i in range(3)])
            junkA = mk(es, "junkA", [128, D], BF16)

            def load4(sl):
                t = xb.next(); m = mxb.next()
                tk.dma("sp", lambda e: e.dma_start(out=t[:], in_=xq[sl * 128:(sl + 1) * 128, :]), [], [t], t)
                tk.dma("sp", lambda e: e.dma_start(out=m[:], in_=MIX_d[sl * 128:(sl + 1) * 128, :]), [R_MIX], [m], m)
                return t, m

            def front(sl, st):
                xt, mt = load4(sl)
                yield
                for c in range(NCH):
                    tk.op("pe", lambda e, c=c: e.transpose(out=tps[:, c * 128:(c + 1) * 128], in_=mt[:, c * 128:(c + 1) * 128], identity=ident[:]), [mt, ident], [tps])
                evac(mT[:], tps[:].rearrange("p (c t) -> p c t", c=NCH), [tps], [mT])
                for n in range(2):
                    for c in range(NCH):
                        tk.op("pe", lambda e, n=n, c=c: e.matmul(wps[n][:], lhsT=mT[:, c, :], rhs=Wo[:, c, n * 512:(n + 1) * 512], start=(c == 0), stop=(c == NCH - 1)), [mT, Wo], [wps[n]])
                x1_ = x1.next()
                for n in range(2):
                    tk.op("dve", lambda e, n=n: e.tensor_tensor(out=x1_[:, n * 512:(n + 1) * 512], in0=wps[n][:], in1=xt[:, n * 512:(n + 1) * 512], op=ALU.add), [wps[n], xt], [x1_])
                yield
                h2_ = h2.next()
                rmsnorm_rows(x1_[:], x1_, junk, ssr.next(), rtr.next(), h2_[:], h2_, gain_bc=gffn)
                st["x1"] = x1_; st["h2"] = h2_
                yield
                for c in range(NCH):
                    tk.op("pe", lambda e, c=c: e.transpose(out=tps[:, c * 128:(c + 1) * 128], in_=h2_[:, c * 128:(c + 1) * 128], identity=ident[:]), [h2_, ident], [tps])
                evac(h2T[:], tps[:].rearrange("p (c t) -> p c t", c=NCH), [tps], [h2T])
                for q4 in range(4):
                    p = qsp.next()
                    for gi in range(4):
                        g = q4 * 4 + gi
                        for c in range(NCH):
                            tk.op("pe", lambda e, g=g, gi=gi, c=c: e.matmul(p[:, gi * 128:(gi + 1) * 128], lhsT=Wq[:, c, g * 128:(g + 1) * 128], rhs=h2T[:, c, :], start=(c == 0), stop=(c == NCH - 1)), [Wq, h2T], [p])
                    evac(qg[:, q4 * 4:(q4 + 1) * 4, :], p[:].rearrange("p (g t) -> p g t", g=4), [p], [qg])
                    yield
                for q4 in range(4):
                    p = qsp.next()
                    for gi in range(4):
                        g = q4 * 4 + gi
                        tk.op("pe", lambda e, g=g, gi=gi: e.matmul(p[:, gi * 128:(gi + 1) * 128], lhsT=qg[:, g, :], rhs=kT[:, g, :], start=True, stop=True), [qg, kT], [p])
                    evac(sc[:, q4 * 4:(q4 + 1) * 4, :], p[:].rearrange("p (g n) -> p g n", g=4), [p], [sc])
                    yield
                for g in range(16):
                    tk.op("dve", lambda e: e.max(out=tops[:, g, 0:8], in_=sc[:, g, :]), [sc], [tops])
                    tk.op("dve", lambda e: e.max_index(out=topi[:, g, 0:8], in_max=tops[:, g, 0:8], in_values=sc[:, g, :]), [sc, tops], [topi])
                    tk.op("dve", lambda e: e.match_replace(out=sc2[:, g, :], in_to_replace=tops[:, g, 0:8], in_values=sc[:, g, :], imm_value=NEG), [sc, tops], [sc2])
                    tk.op("dve", lambda e: e.max(out=tops[:, g, 8:16], in_=sc2[:, g, :]), [sc2], [tops])
                    tk.op("dve", lambda e: e.max_index(out=topi[:, g, 8:16], in_max=tops[:, g, 8:16], in_values=sc2[:, g, :]), [sc2, tops], [topi])
                    if g % 2 == 1:
                        yield
                tk.op("dve", lambda e: e.tensor_copy(out=topf[:], in_=topi[:]), [topi], [topf])
                tv = tops[:].rearrange("p (h c) k -> p h c k", c=2)
                tfv = topf[:].rearrange("p (h c) k -> p h c k", c=2)
                cv = cand[:].rearrange("p h (a b) -> p h a b", a=16)
                tk.op("dve", lambda e: e.tensor_tensor(out=cv, in0=tv[:, :, 0, :].unsqueeze(3).broadcast_to([128, 8, 16, 16]), in1=tv[:, :, 1, :].unsqueeze(2).broadcast_to([128, 8, 16, 16]), op=ALU.add), [tops], [cand])
                yield
                for hh in range(8):
                    tk.op("dve", lambda e: e.max(out=best[:, hh, 0:8], in_=cand[:, hh, :]), [cand], [best])
                    tk.op("dve", lambda e: e.max_index(out=pos[:, hh, 0:8], in_max=best[:, hh, 0:8], in_values=cand[:, hh, :]), [cand, best], [pos])
                    tk.op("dve", lambda e: e.match_replace(out=cand2[:, hh, :], in_to_replace=best[:, hh, 0:8], in_values=cand[:, hh, :], imm_value=NEG), [cand, best], [cand2])
                    tk.op("dve", lambda e: e.max(out=best[:, hh, 8:16], in_=cand2[:, hh, :]), [cand2], [best])
                    tk.op("dve", lambda e: e.max_index(out=pos[:, hh, 8:16], in_max=best[:, hh, 8:16], in_values=cand2[:, hh, :]), [cand2, best], [pos])
                    if hh % 2 == 1:
                        yield
                tk.op("dve", lambda e: e.tensor_single_scalar(out=posa[:], in_=pos[:], scalar=4, op=ALU.logical_shift_right), [pos], [posa])
                tk.op("dve", lambda e: e.tensor_single_scalar(out=posb[:], in_=pos[:], scalar=15, op=ALU.bitwise_and), [pos], [posb])
                tk.op("dve", lambda e: e.tensor_copy(out=paf[:], in_=posa[:]), [posa], [paf])
                tk.op("dve", lambda e: e.tensor_copy(out=pbf_[:], in_=posb[:]), [posb], [pbf_])
                yield
                io = iota16[:].unsqueeze(1).unsqueeze(1).broadcast_to([128, 8, 16, 16])
                for (pf, cc, dst) in ((paf, 0, i1s), (pbf_, 1, i2s)):
                    tk.op("dve", lambda e: e.tensor_tensor(out=eq[:], in0=io, in1=pf[:].unsqueeze(3).broadcast_to([128, 8, 16, 16]), op=ALU.is_equal), [iota16, pf], [eq])
                    yield
                    tk.op("dve", lambda e: e.tensor_tensor(out=eq[:], in0=eq[:], in1=tfv[:, :, cc, :].unsqueeze(2).broadcast_to([128, 8, 16, 16]), op=ALU.mult), [eq, topf], [eq])
                    yield
                    tk.op("dve", lambda e: e.tensor_reduce(out=dst[:], in_=eq[:], axis=AX.X, op=ALU.add), [eq], [dst])
                    yield
                eidx = eidxr.next(); gate = gater.next()
                tk.op("dve", lambda e: e.scalar_tensor_tensor(out=eidf[:], in0=i1s[:].rearrange("p h k -> p (h k)"), scalar=128.0, in1=i2s[:].rearrange("p h k -> p (h k)"), op0=ALU.mult, op1=ALU.add), [i1s, i2s], [eidf])
                tk.op("dve", lambda e: e.tensor_copy(out=eidx[:], in_=eidf[:]), [eidf], [eidx])
                tk.op("dve", lambda e: e.tensor_tensor(out=gate[:], in0=best[:], in1=best[:, :, 0:1].broadcast_to([128, 8, 16]), op=ALU.subtract), [best], [gate])
                tk.op("act", lambda e: e.activation(out=gate[:], in_=gate[:], func=AF.Exp), [gate], [gate])
                tk.op("dve", lambda e: e.tensor_reduce(out=gsum[:], in_=gate[:], axis=AX.X, op=ALU.add), [gate], [gsum])
                tk.op("dve", lambda e: e.reciprocal(out=gsum[:], in_=gsum[:]), [gsum], [gsum])
                tk.op("dve", lambda e: e.tensor_tensor(out=gate[:], in0=gate[:], in1=gsum[:].unsqueeze(2).broadcast_to([128, 8, 16]), op=ALU.mult), [gate, gsum], [gate])
                st["eidx"] = eidx; st["gate"] = gate

            def back(sl, st, gen):
                eidx = st["eidx"]; gate = st["gate"]; h2_ = st["h2"]; x1_ = st["x1"]
                ab = sl % 2
                gflat = gate[:].rearrange("p h k -> p (h k)")
                LAG = 3
                uvs = {}
                for s in range(128 + LAG):
                    if s < 128:
                        uv = uvb.next(); uvs[s] = uv
                        tk.dma("pool", lambda e: e.indirect_dma_start(out=uv[:], out_offset=None, in_=UV_d, in_offset=bass.IndirectOffsetOnAxis(ap=eidx[:, s:s + 1], axis=0)), [eidx, R_UV], [uv], uv)
                        pr_ = prodb.next()
                        tk.op("dve", lambda e: e.tensor_tensor(out=pr_[:], in0=uv[:, 0:D], in1=h2_[:], op=ALU.mult), [uv, h2_], [pr_])
                        tk.op("act", lambda e: e.activation(out=junkA[:], in_=pr_[:], func=AF.Copy, accum_out=actr[ab][:, s:s + 1]), [pr_], [actr_regs[ab][s]])
                        tk.op("act", lambda e: e.activation(out=glr[ab][:, s:s + 1], in_=actr[ab][:, s:s + 1], func=AF.Gelu), [actr_regs[ab][s]], [glr_regs[ab][s]])
                    r = s - LAG
                    if r >= 0:
                        uvr = uvs.pop(r)
                        d_ = dg.next()
                        tk.op("dve", lambda e: e.tensor_scalar(out=d_[:], in0=ident[:], scalar1=glr[ab][:, r:r + 1], scalar2=gflat[:, r:r + 1], op0=ALU.mult, op1=ALU.mult), [ident, glr_regs[ab][r], gate], [d_])
                        for n in range(2):
                            tk.op("pe", lambda e, n=n: e.matmul(yps[n][:], lhsT=d_[:], rhs=uvr[:, D + n * 512:D + (n + 1) * 512], start=(r == 0), stop=(r == 127)), [d_, uvr], [yps[n]])
                    if gen is not None and s % 4 == 3:
                        next(gen, None)
                if gen is not None:
                    for _ in gen:
                        pass
                for n in range(2):
                    tk.op("dve", lambda e, n=n: e.tensor_tensor(out=x1_[:, n * 512:(n + 1) * 512], in0=yps[n][:], in1=x1_[:, n * 512:(n + 1) * 512], op=ALU.add), [yps[n], x1_], [x1_])
                tk.dma("sp", lambda e: e.dma_start(out=out[sl * 128:(sl + 1) * 128, :], in_=x1_[:]), [x1_], [R_OUT], x1_)

            states = [dict() for _ in range(NSLOT)]
            evac_act_only[0] = True
            for _ in front(0, states[0]):
                pass
            for sl in range(NSLOT):
                gen = front(sl + 1, states[sl + 1]) if sl + 1 < NSLOT else None
                back(sl, states[sl], gen)
            tk.barrier()

        tk.barrier()
    return nc


def host_inputs(inputs, SEQ, cores):
    x = np.asarray(inputs["x"], np.float32)
    NBLK = SEQ // 128
    f32 = np.float32

    def col8(v):
        return np.ascontiguousarray(np.asarray(v, f32).reshape(8, 128).T)
    g_out = np.concatenate([np.asarray(inputs["sb_out_gain"], f32).reshape(-1),
                            np.asarray(inputs["hg_out_gain"], f32).reshape(-1),
                            np.ones(256, f32)])
    gam = np.asarray(inputs["gamma_lb"], f32).reshape(2, 4, 128).transpose(2, 0, 1)
    keys = np.asarray(inputs["peer_sub_keys"], f32).reshape(16, 128, 128)
    shared = {
        "w_in": np.ascontiguousarray(np.asarray(inputs["w_in"], f32)[0]),
        "w_out": np.ascontiguousarray(np.asarray(inputs["w_out"], f32)[0]),
        "w_kv": np.ascontiguousarray(np.asarray(inputs["w_mem_kv"], f32)[0]),
        "w_pq": np.ascontiguousarray(np.asarray(inputs["peer_w_query"], f32)[0]),
        "keysT": np.ascontiguousarray(keys.transpose(2, 0, 1)),
        "peer_u": np.ascontiguousarray(np.asarray(inputs["peer_u"], f32)[0]),
        "peer_v": np.ascontiguousarray(np.asarray(inputs["peer_v"], f32)[0]),
        "g_mix": col8(np.asarray(inputs["norm_mix_gain"])[0]),
        "g_mem": col8(inputs["mem_norm_gain"]),
        "g_ffn_bc": np.ascontiguousarray(np.broadcast_to(np.asarray(inputs["norm_ffn_gain"], f32)[0][None, :], (128, D))),
        "g_out": col8(g_out),
        "gam": np.ascontiguousarray(gam),
        "g_mq": np.ascontiguousarray(np.tile(np.asarray(inputs["mem_q_gain"], f32)[0], 2).reshape(128, 1)),
        "g_mk": np.ascontiguousarray(np.tile(np.asarray(inputs["mem_k_gain"], f32)[0], 2).reshape(128, 1)),
        "c_ident": np.eye(128, dtype=f32),
        "c_maskst": np.triu(np.ones((128, 128), f32)),
        "c_reset": np.ascontiguousarray(np.broadcast_to((np.arange(512) % 64 != 0).astype(f32)[None, :], (128, 512))),
        "c_iota16": np.ascontiguousarray(np.broadcast_to(np.arange(16, dtype=f32)[None, :], (128, 16))),
    }
    tri = (np.arange(128)[None, :] >= np.arange(128)[:, None]).astype(f32)
    allm = np.ones((128, 128), f32)
    none = np.zeros((128, 128), f32)
    A = np.concatenate([tri, allm], axis=1)
    Bm = np.concatenate([none, tri], axis=1)
    maps = []
    for core in cores:
        b, c = core // 2, core % 2
        blocks = own_blocks(c, NBLK)
        xb = x[b, :SEQ].reshape(NBLK, 128, D)
        m = dict(shared)
        m["xk"] = np.ascontiguousarray(x[b, :SEQ])
        m["xq"] = np.ascontiguousarray(xb[blocks].reshape(-1, D))
        m["mem"] = np.ascontiguousarray(np.asarray(inputs["mem"], f32)[b])
        m["c_maskg"] = np.ascontiguousarray(np.stack([A, Bm] if c == 0 else [Bm, A], axis=1))
        bl = np.array([1, 0, 0, 1] if c == 0 else [0, 1, 1, 0], f32)
        m["c_blend"] = np.ascontiguousarray(np.broadcast_to(bl[None, :], (128, 4)))
        maps.append(m)
    return maps


_NC_CACHE = {}


def kernel(**inputs):
    x = np.asarray(inputs["x"])
    B, SEQ, _ = x.shape
    cores = list(range(2 * B))
    if SEQ not in _NC_CACHE:
        _NC_CACHE[SEQ] = build(SEQ)
    nc = _NC_CACHE[SEQ]
    maps = host_inputs(inputs, SEQ, cores)
    res = run_bass_kernel_spmd(nc, maps, core_ids=cores)
    NBLK = SEQ // 128
    outp = np.zeros((B, SEQ, D), np.float32)
    ov = outp.reshape(B, NBLK, 128, D)
    for core in cores:
        b, c = core // 2, core % 2
        blocks = own_blocks(c, NBLK)
        ov[b, blocks] = np.asarray(res.results[core]["out"], np.float32).reshape(len(blocks), 128, D)
    return outp
```
